# Optimizing a Trainium2 kernel written in Bass

```python
import math
import jax, jax.numpy as jnp
from jax import lax
import numpy as np

D_MODEL = 2048
BATCH = 1
SEQ = 8192
DEPTH = 1

D_MIX = D_MODEL
D_ATTN = D_MIX // 2
D_CONV = D_MIX - D_ATTN
HEAD_DIM = 128
N_HEADS = D_ATTN // HEAD_DIM
CONV_GROUPS = 8
MOBA_BLOCK = 256
MOBA_TOPK = 3
QUERY_CHUNK = 64
CONV_WIDTH = 31
N_EXPERTS = 32
TOP_K = 4
D_FF = D_MODEL
SWIGLU_ALPHA = 1.702
SWIGLU_LIMIT = 7.0
ROUTE_BLOCK = 128
LN_EPS = 1e-5
DEEPNORM_ALPHA = (2.0 * DEPTH) ** 0.25
DEEPNORM_BETA = (8.0 * DEPTH) ** -0.25
IN_COLS = 3 * D_ATTN + 2 * D_CONV

kernel_name = "hybrid_moba_conformer_moe_deepnorm"


def layer_norm(x, g, b):
    xf = x.astype(jnp.float32)
    mu = jnp.mean(xf, axis=-1, keepdims=True)
    var = jnp.mean(jnp.square(xf - mu), axis=-1, keepdims=True)
    y = (xf - mu) * lax.rsqrt(var + LN_EPS) * g.astype(jnp.float32) + b.astype(jnp.float32)
    return y.astype(x.dtype)


def alibi_slopes(n_heads):
    return 2.0 ** (-(8.0 / n_heads) * jnp.arange(1, n_heads + 1, dtype=jnp.float32))


def moba_attention(q, k, v):
    B, S, H, hd = q.shape
    blk = MOBA_BLOCK
    nb = -(-S // blk)
    s_pad = nb * blk
    pad = ((0, 0), (0, s_pad - S), (0, 0), (0, 0))
    kb = jnp.pad(k, pad).reshape(B, nb, blk, H, hd).transpose(0, 3, 1, 2, 4)
    vb = jnp.pad(v, pad).reshape(B, nb, blk, H, hd).transpose(0, 3, 1, 2, 4)
    k_mean = jnp.mean(kb.astype(jnp.float32), axis=3)

    q_blk = jnp.arange(S) // blk
    gate = jnp.einsum('bshd,bhnd->bhsn', q.astype(jnp.float32), k_mean)
    fully_past = jnp.arange(nb)[None, :] < q_blk[:, None]
    gate = jnp.where(fully_past[None, None], gate, -jnp.inf)
    n_sel = min(MOBA_TOPK, nb)
    sel_score, sel = lax.top_k(gate, n_sel)
    sel_valid = sel_score > -jnp.inf

    qh = q.transpose(0, 2, 1, 3)
    slopes = alibi_slopes(H)
    scale = hd ** -0.5
    b_ix = jnp.arange(B)[:, None, None, None]
    h_ix = jnp.arange(H)[None, :, None, None]
    qc = QUERY_CHUNK

    def chunk(c):
        t0 = c * qc
        q_c = lax.dynamic_slice_in_dim(qh, t0, qc, axis=2)
        sel_c = lax.dynamic_slice_in_dim(sel, t0, qc, axis=2)
        val_c = lax.dynamic_slice_in_dim(sel_valid, t0, qc, axis=2)
        t = t0 + jnp.arange(qc)
        k_sel = kb[b_ix, h_ix, sel_c]
        v_sel = vb[b_ix, h_ix, sel_c]
        s_sel = jnp.einsum('bhqd,bhqnkd->bhqnk', q_c, k_sel).astype(jnp.float32) * scale
        pos_sel = sel_c[..., None] * blk + jnp.arange(blk)
        dist_sel = (t[None, None, :, None, None] - pos_sel).astype(jnp.float32)
        s_sel = s_sel - slopes[None, :, None, None, None] * dist_sel
        s_sel = jnp.where(val_c[..., None], s_sel, -jnp.inf)
        own = t0 // blk
        k_own = lax.dynamic_index_in_dim(kb, own, axis=2, keepdims=False)
        v_own = lax.dynamic_index_in_dim(vb, own, axis=2, keepdims=False)
        s_own = jnp.einsum('bhqd,bhkd->bhqk', q_c, k_own).astype(jnp.float32) * scale
        dist_own = t[:, None] - (own * blk + jnp.arange(blk))[None, :]
        s_own = jnp.where(dist_own[None, None] >= 0,
                          s_own - slopes[None, :, None, None] * dist_own.astype(jnp.float32)[None, None],
                          -jnp.inf)
        scores = jnp.concatenate([s_sel.reshape(B, H, qc, n_sel * blk), s_own], axis=-1)
        p = jax.nn.softmax(scores, axis=-1).astype(v.dtype)
        p_sel = p[..., :n_sel * blk].reshape(B, H, qc, n_sel, blk)
        p_own = p[..., n_sel * blk:]
        return (jnp.einsum('bhqnk,bhqnkd->bhqd', p_sel, v_sel)
                + jnp.einsum('bhqk,bhkd->bhqd', p_own, v_own))

    outs = lax.map(chunk, jnp.arange(S // qc))
    return outs.transpose(1, 0, 3, 2, 4).reshape(B, S, H * hd)


def conformer_conv(ga, gb, conv_w, conv_b, conv_ln_g, conv_ln_b):
    u = ga * jax.nn.sigmoid(gb)
    c = lax.conv_general_dilated(
        u, conv_w[:, None, :], window_strides=(1,), padding=[(CONV_WIDTH - 1, 0)],
        dimension_numbers=('NWC', 'WIO', 'NWC'), feature_group_count=u.shape[-1]) + conv_b
    c = layer_norm(c, conv_ln_g, conv_ln_b)
    return c * jax.nn.sigmoid(c)


def moe_ffn(h, w_router, b_router, w_gate_up, b_gate_up, w_down, b_down):
    B, S, D = h.shape
    T = B * S
    xt = h.reshape(T, D)
    logits = (xt @ w_router + b_router).astype(jnp.float32)
    top_val, top_idx = lax.top_k(logits, TOP_K)
    gates = jax.nn.softmax(top_val, axis=-1)
    A = T * TOP_K
    flat_e = top_idx.reshape(A)
    flat_tok = jnp.arange(A, dtype=jnp.int32) // TOP_K
    flat_g = gates.reshape(A)
    order = jnp.argsort(flat_e)
    e_sorted = flat_e[order]
    counts = jnp.bincount(flat_e, length=N_EXPERTS)
    starts = jnp.cumsum(counts) - counts
    padded = ((counts + ROUTE_BLOCK - 1) // ROUTE_BLOCK) * ROUTE_BLOCK
    pad_end = jnp.cumsum(padded)
    pad_start = pad_end - padded
    dest = pad_start[e_sorted] + (jnp.arange(A) - starts[e_sorted])
    n_blocks = -(-A // ROUTE_BLOCK) + N_EXPERTS
    cap = n_blocks * ROUTE_BLOCK
    row_tok = jnp.zeros((cap,), jnp.int32).at[dest].set(flat_tok[order])
    row_gate = jnp.zeros((cap,), h.dtype).at[dest].set(flat_g[order].astype(h.dtype))
    block_e = jnp.minimum(
        jnp.searchsorted(pad_end, jnp.arange(n_blocks) * ROUTE_BLOCK, side='right'),
        N_EXPERTS - 1)

    def run_block(args):
        tok, e = args
        xb = xt[tok]
        gu = xb @ w_gate_up[e] + b_gate_up[e]
        g, u = gu[:, :D_FF], gu[:, D_FF:]
        g = jnp.minimum(g, SWIGLU_LIMIT)
        u = jnp.clip(u, -SWIGLU_LIMIT, SWIGLU_LIMIT)
        act = (u + 1.0) * (g * jax.nn.sigmoid(SWIGLU_ALPHA * g))
        return act @ w_down[e] + b_down[e]

    y_rows = lax.map(run_block, (row_tok.reshape(n_blocks, ROUTE_BLOCK), block_e))
    y = jax.ops.segment_sum(y_rows.reshape(cap, D) * row_gate[:, None], row_tok, num_segments=T)
    return y.reshape(B, S, D)


def setup_inputs(seed: int = 0) -> dict:
    key = jax.random.key(seed)
    ks = jax.random.split(key, 24)
    f32 = jnp.float32
    nrm = lambda k, shape, s: jax.random.normal(k, shape, f32) * s
    d_in = D_MODEL ** -0.5
    w_q = nrm(ks[1], (D_MODEL, D_ATTN), d_in)
    w_k = nrm(ks[2], (D_MODEL, D_ATTN), d_in)
    w_v = nrm(ks[3], (D_MODEL, D_ATTN), d_in * DEEPNORM_BETA)
    w_ga = nrm(ks[4], (D_MODEL, D_CONV), d_in)
    w_gb = nrm(ks[5], (D_MODEL, D_CONV), d_in)
    return {
        "x": jax.random.normal(ks[0], (BATCH, SEQ, D_MODEL), f32),
        "w_in": jnp.concatenate([w_q, w_k, w_v, w_ga, w_gb], axis=1),
        "conv_w": nrm(ks[6], (CONV_WIDTH, D_CONV), CONV_WIDTH ** -0.5),
        "conv_b": nrm(ks[7], (D_CONV,), 0.01),
        "conv_ln_g": 1.0 + nrm(ks[8], (D_CONV,), 0.01),
        "conv_ln_b": nrm(ks[9], (D_CONV,), 0.01),
        "w_out": nrm(ks[10], (D_MIX, D_MODEL), D_MIX ** -0.5 * DEEPNORM_BETA),
        "ln1_g": 1.0 + nrm(ks[11], (D_MODEL,), 0.01),
        "ln1_b": nrm(ks[12], (D_MODEL,), 0.01),
        "w_router": nrm(ks[13], (D_MODEL, N_EXPERTS), d_in),
        "b_router": nrm(ks[14], (N_EXPERTS,), 0.01),
        "w_gate_up": nrm(ks[15], (N_EXPERTS, D_MODEL, 2 * D_FF), d_in * DEEPNORM_BETA),
        "b_gate_up": nrm(ks[16], (N_EXPERTS, 2 * D_FF), 0.01),
        "w_down": nrm(ks[17], (N_EXPERTS, D_FF, D_MODEL), D_FF ** -0.5 * DEEPNORM_BETA),
        "b_down": nrm(ks[18], (N_EXPERTS, D_MODEL), 0.01),
        "ln2_g": 1.0 + nrm(ks[19], (D_MODEL,), 0.01),
        "ln2_b": nrm(ks[20], (D_MODEL,), 0.01),
    }


def reference(x, w_in, conv_w, conv_b, conv_ln_g, conv_ln_b, w_out, ln1_g, ln1_b,
              w_router, b_router, w_gate_up, b_gate_up, w_down, b_down, ln2_g, ln2_b):
    B, S, _ = x.shape
    h = x
    for _layer in range(DEPTH):
        proj = h @ w_in
        q, k, v, ga, gb = jnp.split(
            proj, [D_ATTN, 2 * D_ATTN, 3 * D_ATTN, 3 * D_ATTN + D_CONV], axis=-1)
        q = q.reshape(B, S, N_HEADS, HEAD_DIM)
        k = k.reshape(B, S, N_HEADS, HEAD_DIM)
        v = v.reshape(B, S, N_HEADS, HEAD_DIM)
        attn = moba_attention(q, k, v)
        conv = conformer_conv(ga, gb, conv_w, conv_b, conv_ln_g, conv_ln_b)
        mix = jnp.concatenate([attn, conv], axis=-1) @ w_out
        h = layer_norm(DEEPNORM_ALPHA * h + mix, ln1_g, ln1_b)
        ffn = moe_ffn(h, w_router, b_router, w_gate_up, b_gate_up, w_down, b_down)
        h = layer_norm(DEEPNORM_ALPHA * h + ffn, ln2_g, ln2_b)
    return h
```

```python
import os
import numpy as np
from contextlib import ExitStack
import concourse.bass as bass
import concourse.mybir as mybir
from concourse.bass_utils import run_bass_kernel_spmd

F32 = mybir.dt.float32
BF16 = mybir.dt.bfloat16
AF = mybir.ActivationFunctionType
ALU = mybir.AluOpType
AX = mybir.AxisListType

NCORES = 8
SEQ = 8192
DM = 2048
TOK = 1024
NT = 8
NH = 8
NE = 32
CAP = 192
MJ = (128, CAP - 128)
ALPHA = 2.0 ** 0.25
SCALE = 128.0 ** -0.5
SLOPES = [2.0 ** (-(h + 1)) for h in range(NH)]
EPS = 1e-5
NEG = -1.0e30


class _Stop(Exception):
    pass


class _NoExc:
    def __init__(self, cm):
        self.cm = cm

    def __enter__(self):
        return self.cm.__enter__()

    def __exit__(self, *a):
        self.cm.__exit__(None, None, None)
        return False


class Sched:
    ENGS = ('pe', 'act', 'dve', 'pool', 'sp')
    NS = 6

    def __init__(self, nc):
        self.nc = nc
        self.streams = {e: [] for e in self.ENGS}
        self.lastw = {}
        self.readers = {}
        self.dma_since = []

    def _add(self, eng, fn, r, w, dma):
        idx = len(self.streams[eng])
        node = (eng, idx)
        deps = set()
        for k in r:
            lw = self.lastw.get(k)
            if lw is not None:
                deps.add(lw)
        for k in w:
            lw = self.lastw.get(k)
            if lw is not None and (lw[0] != eng or self.streams[lw[0]][lw[1]]['dma'] or dma):
                deps.add(lw)
            for rd in self.readers.get(k, ()):
                if rd[0] != eng or self.streams[rd[0]][rd[1]]['dma'] or dma:
                    deps.add(rd)
        deps.discard(node)
        rec = dict(fn=fn, deps=deps, dma=dma, has_dep=False)
        self.streams[eng].append(rec)
        for d in deps:
            self.streams[d[0]][d[1]]['has_dep'] = True
        for k in w:
            self.lastw[k] = node
            self.readers[k] = []
        for k in r:
            lst = self.readers.setdefault(k, [])
            if not dma:
                for j in range(len(lst)):
                    if lst[j][0] == eng and not self.streams[eng][lst[j][1]]['dma']:
                        lst[j] = node
                        break
                else:
                    lst.append(node)
            else:
                lst.append(node)
        if dma:
            self.dma_since.append(node)
        return node

    def op(self, eng, meth, r, w, *a, **k):
        return self._add(eng, lambda e: getattr(e, meth)(*a, **k), tuple(r), tuple(w), False)

    def dma(self, q, r, w, **k):
        return self._add(q, lambda e: e.dma_start(**k), tuple(r), tuple(w), True)

    def barrier(self):
        deps = set(self.dma_since)
        for e in self.ENGS:
            st = self.streams[e]
            for i in range(len(st) - 1, -1, -1):
                if st[i]['fn'] is not None and not st[i]['dma']:
                    deps.add((e, i))
                    break
        for d in deps:
            self.streams[d[0]][d[1]]['has_dep'] = True
        for e in self.ENGS:
            self.streams[e].append(dict(fn=None, deps=set(deps), dma=False, has_dep=False))
        self.dma_since = []
        self.lastw = {}
        self.readers = {}

    def finalize(self, stack):
        nc = self.nc
        esem = {e: stack.enter_context(nc.semaphore("es_" + e)) for e in self.ENGS}
        dsem = {e: [stack.enter_context(nc.semaphore("ds_%s%d" % (e, i))) for i in range(self.NS)]
                for e in ('sp', 'act', 'pool')}
        ndma = {}
        for e in self.ENGS:
            tick = 0
            nd = 0
            for rec in self.streams[e]:
                if rec['fn'] is None:
                    continue
                if rec['dma']:
                    s = dsem[e][nd % self.NS]
                    rec['tok'] = (s, 16 * (nd // self.NS + 1))
                    rec['pre'] = (s, 16 * (nd // self.NS))
                    nd += 1
                elif rec['has_dep']:
                    tick += 1
                    rec['tok'] = (esem[e], tick)
            ndma[e] = nd
        block = stack.enter_context(nc.Block())
        streams = self.streams
        NS = self.NS

        def emit(e, engobj):
            waited = {}

            def wait(tok):
                s, v = tok
                if v <= 0:
                    return
                key = id(s)
                if waited.get(key, (None, 0))[1] >= v:
                    return
                waited[key] = (s, v)
                engobj.wait_ge(s, v)
            for rec in streams[e]:
                for d in sorted(rec['deps']):
                    if d[0] == e and not streams[d[0]][d[1]]['dma'] and rec['fn'] is None:
                        continue
                    wait(streams[d[0]][d[1]]['tok'])
                if rec['fn'] is None:
                    continue
                if rec['dma']:
                    wait(rec['pre'])
                    rec['fn'](engobj).then_inc(rec['tok'][0], 16)
                else:
                    ins = rec['fn'](engobj)
                    if rec['has_dep']:
                        ins.then_inc(rec['tok'][0], 1)
            if e == 'sp':
                for q in ('sp', 'act', 'pool'):
                    nd = ndma[q]
                    for i in range(min(nd, NS)):
                        cnt = (nd - 1 - i) // NS + 1
                        wait((dsem[q][i], 16 * cnt))

        @block.tensor
        def _(eng):
            emit('pe', eng)

        @block.scalar
        def _(eng):
            emit('act', eng)

        @block.vector
        def _(eng):
            emit('dve', eng)

        @block.gpsimd
        def _(eng):
            emit('pool', eng)

        @block.sync
        def _(eng):
            emit('sp', eng)


def build(stop=99):
    nc = bass.Bass("TRN2", target_bir_lowering=False)

    def din(name, shape, dt=F32):
        return nc.dram_tensor(name, list(shape), dt, kind="ExternalInput").ap()

    xT = din("xT", [DM, SEQ])
    xTo = din("xTo", [DM, 1056])
    xo = din("xo", [TOK, DM])
    w_in = din("w_in", [DM, 5120])
    convw = din("convw", [128, 8 * 31])
    convv = din("convv", [128, 24])
    w_out = din("w_out", [DM, DM])
    ln1 = din("ln1", [2, DM])
    ln2 = din("ln2", [2, DM])
    w_r = din("w_r", [DM, NE])
    b_r = din("b_r", [1, NE])
    w_gu = din("w_gu", [NE, DM, 2 * DM]) if stop >= 7 else None
    bgu = din("bgu", [128, NE * 32])
    w_dn = din("w_dn", [NE, DM, DM]) if stop >= 7 else None
    b_dn = din("b_dn", [NE, DM])
    cst = din("cst", [128, 1024])
    cst2 = din("cst2", [128, 64])
    dtab = din("dtab", [128, 8 * 32])
    fptab = din("fptab", [128, 8 * 32])
    out = nc.dram_tensor("out", [TOK, DM], F32, kind="ExternalOutput").ap()
    if stop < 99:
        dbgf = nc.dram_tensor("dbgf", [128, 16384], F32, kind="ExternalOutput").ap()
        dbgb = nc.dram_tensor("dbgb", [128, 16384], BF16, kind="ExternalOutput").ap()
    KTs = nc.dram_tensor("KTs", [NH, 128, SEQ], BF16, kind="Internal").ap()
    Vs = nc.dram_tensor("Vs", [SEQ, 1024], BF16, kind="Internal").ap()
    RK = nc.dram_tensor("RK", [NE, TOK], F32, kind="Internal").ap()
    GT = nc.dram_tensor("GT", [NE, TOK], F32, kind="Internal").ap()

    with ExitStack() as st:
        S = Sched(nc)

        def dump_f(ap, off, n):
            S.dma('sp', [], [('dbgf', off)], out=dbgf[:, off:off + n], in_=ap)

        def sb(stack, name, shape, dt=F32):
            return stack.enter_context(_NoExc(nc.sbuf_tensor(name, list(shape), dt)))

        PS = [st.enter_context(nc.psum_tensor("ps%d" % i, [128, 512], F32)) for i in range(8)]

        cf = sb(st, "cf", [128, 1024])
        cb = sb(st, "cb", [128, 512], BF16)
        c2 = sb(st, "c2", [128, 64])
        S.dma('sp', [], ['cf'], out=cf[:], in_=cst[:, :])
        S.dma('sp', [], ['c2'], out=c2[:], in_=cst2[:, :])
        S.op('dve', 'tensor_copy', ['cf'], ['cb'], out=cb[:], in_=cf[:, 0:512])
        identf = cf[:, 0:128]
        onesf = cf[:, 384:512]
        iota_row = cf[:, 512:768]
        identb = cb[:, 0:128]
        trib = cb[:, 128:256]
        utrib = cb[:, 256:384]
        onesb = cb[:, 384:512]
        iota_p2 = c2[:, 0:2]
        kbias = c2[:, 2:18]
        down = c2[:, 18:26]

        accraw = sb(st, "accraw", [128, 2 * NT * DM], BF16)
        acc = accraw[:].bitcast(F32).rearrange("p (a d) -> p a d", d=DM)
        S.barrier()

        try:
            with ExitStack() as s1:
                convT = sb(s1, "convT", [128, 8, TOK], BF16)
                kmean = sb(s1, "kmean", [128, NH, 32])
                w_in_v = w_in.rearrange("(c p) f -> p c f", p=128)
                accf = accraw[:].bitcast(F32)
                accb = accraw[:]

                with ExitStack() as sc:
                    wga = [sb(sc, "wga%d" % i, [128, 16, 128], BF16) for i in range(2)]
                    wgb = [sb(sc, "wgb%d" % i, [128, 16, 128], BF16) for i in range(2)]
                    cw = sb(sc, "cw", [128, 8, 31])
                    cv = sb(sc, "cv", [128, 24])
                    sg = [sb(sc, "sg%d" % i, [128, 1056]) for i in range(2)]
                    ug = [sb(sc, "ug%d" % i, [128, 1056]) for i in range(2)]
                    sq = [sb(sc, "sq%d" % i, [128, 1024]) for i in range(2)]
                    xto = sb(sc, "xto_c", [128, 16, 1056], BF16)
                    S.dma('pool', [], ['xto'], out=xto[:], in_=xTo.rearrange("(c p) t -> p c t", p=128))
                    cT = accf[:, 0:8192].rearrange("p (g t) -> p g t", t=TOK)
                    ty = [accf[:, 8192 + i * 1024:8192 + (i + 1) * 1024] for i in range(2)]
                    tz = [accf[:, 10240 + i * 1024:10240 + (i + 1) * 1024] for i in range(2)]
                    mean = accf[:, 12288:13312]
                    rstd = accf[:, 13312:14336]
                    msq = accf[:, 14336:15360]
                    S.dma('sp', [], ['cw'], out=cw[:], in_=convw.rearrange("p (g j) -> p g j", j=31))
                    S.dma('sp', [], ['cv'], out=cv[:], in_=convv[:, :])
                    for g in range(8):
                        b = g % 2
                        S.dma('pool', [], [('wga', b)], out=wga[b][:], in_=w_in_v[:, :, 3072 + g * 128:3072 + (g + 1) * 128])
                        S.dma('pool', [], [('wgb', b)], out=wgb[b][:], in_=w_in_v[:, :, 4096 + g * 128:4096 + (g + 1) * 128])
                        for k in range(3):
                            pa = k % 2
                            pbb = 2 + k % 2
                            cs = slice(k * 352, (k + 1) * 352)
                            for c in range(16):
                                S.op('pe', 'matmul', [('wga', b), 'xto'], [('ps', pa)], PS[pa][:, 0:352],
                                     lhsT=wga[b][:, c, :], rhs=xto[:, c, cs], start=(c == 0), stop=(c == 15))
                            for c in range(16):
                                S.op('pe', 'matmul', [('wgb', b), 'xto'], [('ps', pbb)], PS[pbb][:, 0:352],
                                     lhsT=wgb[b][:, c, :], rhs=xto[:, c, cs], start=(c == 0), stop=(c == 15))
                            S.op('act', 'activation', [('ps', pbb)], [('sg', b, k)], out=sg[b][:, cs], in_=PS[pbb][:, 0:352], func=AF.Sigmoid)
                            S.op('dve', 'tensor_tensor', [('ps', pa), ('sg', b, k)], [('ug', b, k)], out=ug[b][:, cs], in0=PS[pa][:, 0:352], in1=sg[b][:, cs], op=ALU.mult)
                        ukeys = [('ug', b, k) for k in range(3)]
                        S.op('dve', 'tensor_scalar', ukeys + ['cw', 'cv'], [('cT', g)], out=cT[:, g, :], in0=ug[b][:, 2:2 + TOK],
                             scalar1=cw[:, g, 0:1], scalar2=cv[:, g:g + 1], op0=ALU.mult, op1=ALU.add)
                        for j in range(1, 31):
                            S.op('dve', 'scalar_tensor_tensor', ukeys + ['cw', ('cT', g)], [('cT', g)], out=cT[:, g, :], in0=ug[b][:, j + 2:j + 2 + TOK],
                                 scalar=cw[:, g, j:j + 1], in1=cT[:, g, :], op0=ALU.mult, op1=ALU.add)
                        S.op('act', 'activation', [('cT', g)], [('sq', b)], out=sq[b][:], in_=cT[:, g, :], func=AF.Square)
                        for half in range(2):
                            hs = slice(half * 512, (half + 1) * 512)
                            S.op('pe', 'matmul', [('cT', g)], [('ps', 4 + half)], PS[4 + half][:], lhsT=onesf, rhs=cT[:, g, hs], start=(g == 0), stop=(g == 7))
                            S.op('pe', 'matmul', [('sq', b)], [('ps', 6 + half)], PS[6 + half][:], lhsT=onesf, rhs=sq[b][:, hs], start=(g == 0), stop=(g == 7))
                    for half in range(2):
                        hs = slice(half * 512, (half + 1) * 512)
                        S.op('act', 'activation', [('ps', 4 + half)], [('mean', half)], out=mean[:, hs], in_=PS[4 + half][:], func=AF.Copy, scale=1.0 / 1024.0)
                        S.op('dve', 'tensor_tensor', [('mean', half)], [('msq', half)], out=msq[:, hs], in0=mean[:, hs], in1=mean[:, hs], op=ALU.mult)
                        S.op('dve', 'scalar_tensor_tensor', [('ps', 6 + half), ('msq', half)], [('rstd', half)], out=rstd[:, hs], in0=PS[6 + half][:],
                             scalar=1.0 / 1024.0, in1=msq[:, hs], op0=ALU.mult, op1=ALU.subtract)
                        S.op('dve', 'tensor_scalar', [('rstd', half)], [('rstd', half)], out=rstd[:, hs], in0=rstd[:, hs], scalar1=EPS, scalar2=None, op0=ALU.add)
                        S.op('act', 'activation', [('rstd', half)], [('rstd', half)], out=rstd[:, hs], in_=rstd[:, hs], func=AF.Sqrt)
                        S.op('dve', 'reciprocal', [('rstd', half)], [('rstd', half)], out=rstd[:, hs], in_=rstd[:, hs])
                    for g in range(8):
                        b = g % 2
                        S.op('dve', 'tensor_tensor', [('cT', g), ('mean', 0), ('mean', 1)], [('ty', b)], out=ty[b][:], in0=cT[:, g, :], in1=mean[:], op=ALU.subtract)
                        S.op('dve', 'tensor_tensor', [('ty', b), ('rstd', 0), ('rstd', 1)], [('ty', b)], out=ty[b][:], in0=ty[b][:], in1=rstd[:], op=ALU.mult)
                        S.op('act', 'activation', [('ty', b), 'cv'], [('tz', b)], out=tz[b][:], in_=ty[b][:], func=AF.Identity, scale=cv[:, 8 + g:9 + g], bias=cv[:, 16 + g:17 + g])
                        S.op('act', 'activation', [('tz', b)], [('ty', b)], out=ty[b][:], in_=tz[b][:], func=AF.Sigmoid)
                        S.op('dve', 'tensor_tensor', [('ty', b), ('tz', b)], [('convT', g)], out=convT[:, g, :], in0=tz[b][:], in1=ty[b][:], op=ALU.mult)
                    S.barrier()
                    if stop == 1:
                        S.dma('sp', [], ['dbgb1'], out=dbgb[:, 0:8192], in_=convT[:].rearrange('p a d -> p (a d)'))
                        raise _Stop()

                with ExitStack() as sa:
                    Wk = accb[:, 0:16384].rearrange("p (c f) -> p c f", f=1024)
                    Wv = accb[:, 16384:32768].rearrange("p (c f) -> p c f", f=1024)
                    for cg in range(4):
                        S.dma('pool', [], [('Wk', cg)], out=Wk[:, cg * 4:(cg + 1) * 4, :], in_=w_in_v[:, cg * 4:(cg + 1) * 4, 1024:2048])
                        S.dma('pool', [], [('Wv', cg)], out=Wv[:, cg * 4:(cg + 1) * 4, :], in_=w_in_v[:, cg * 4:(cg + 1) * 4, 2048:3072])
                    with ExitStack() as sa1:
                        xc = [sb(sa1, "xc%d" % i, [128, 16, 512], BF16) for i in range(2)]
                        ktsb = [sb(sa1, "ktsb%d" % i, [128, 512], BF16) for i in range(3)]
                        vsb = [sb(sa1, "vsb%d" % i, [128, 1024], BF16) for i in range(2)]
                        kmsum = sb(sa1, "kmsum", [128, NH, 32])
                        xT_v = xT.rearrange("(c p) t -> p c t", p=128)
                        ke = 0
                        ve = 0
                        for tcn in range(16):
                            b = tcn % 2
                            S.dma('pool', [], [('xc', b)], out=xc[b][:], in_=xT_v[:, :, tcn * 512:(tcn + 1) * 512])
                            for h in range(NH if not os.environ.get('A_NOK') else 0):
                                pb = h % 2
                                for c in range(16):
                                    S.op('pe', 'matmul', [('Wk', c // 4), ('xc', b)], [('ps', pb)], PS[pb][:],
                                         lhsT=Wk[:, c, h * 128:(h + 1) * 128], rhs=xc[b][:, c, :],
                                         start=(c == 0), stop=(c == 15))
                                kb = ke % 3
                                ke += 1
                                S.op('act', 'activation', [('ps', pb)], [('ktsb', kb)], out=ktsb[kb][:], in_=PS[pb][:], func=AF.Copy)
                                if not os.environ.get('A_NORED'):
                                  S.op('dve', 'tensor_reduce', [('ktsb', kb)], [('kmsum', h, tcn)],
                                     out=kmsum[:, h, 2 * tcn:2 * tcn + 2],
                                     in_=ktsb[kb][:].rearrange("p (b t) -> p b t", t=256), axis=AX.X, op=ALU.add)
                                if not os.environ.get('SKIP_SCR'):
                                  S.dma('sp', [('ktsb', kb)], [('KTs', h, tcn)], out=KTs[h, :, tcn * 512:(tcn + 1) * 512], in_=ktsb[kb][:])
                            for s in range(4 if not os.environ.get('A_NOV') else 0):
                                vb = ve % 2
                                ve += 1
                                for half in range(2):
                                    pb = 2 + half
                                    for c in range(16):
                                        S.op('pe', 'matmul', [('Wv', c // 4), ('xc', b)], [('ps', pb)], PS[pb][:],
                                             lhsT=xc[b][:, c, s * 128:(s + 1) * 128], rhs=Wv[:, c, half * 512:(half + 1) * 512],
                                             start=(c == 0), stop=(c == 15))
                                    if half == 0:
                                        S.op('act', 'activation', [('ps', pb)], [('vsb', vb, half)], out=vsb[vb][:, 0:512], in_=PS[pb][:], func=AF.Copy)
                                    else:
                                        S.op('dve', 'tensor_copy', [('ps', pb)], [('vsb', vb, half)], out=vsb[vb][:, 512:1024], in_=PS[pb][:])
                                t0 = tcn * 512 + s * 128
                                if not os.environ.get('SKIP_SCR'):
                                  S.dma('sp', [('vsb', vb, 0), ('vsb', vb, 1)], [('Vs', t0)], out=Vs[t0:t0 + 128, :], in_=vsb[vb][:])
                        S.op('dve', 'tensor_scalar', [('kmsum', h, t) for h in range(NH) for t in range(16)], ['kmean'],
                             out=kmean[:], in0=kmsum[:], scalar1=1.0 / 256.0, scalar2=None, op0=ALU.mult)
                        S.barrier()
                        if stop == 2:
                            dump_f(kmean[:].rearrange('p a b -> p (a b)'), 2048, 256)
                            raise _Stop()

                    QT = sb(s1, "QT", [128, NH, TOK], BF16)
                    KTo = sb(s1, "KTo", [128, NH, TOK], BF16)
                    Vo = sb(s1, "Vo", [128, NT, NH, 129], BF16)
                    Wt = sb(s1, "Wt", [128, NH, NT, 32])
                    with ExitStack() as sa2:
                        xto = sb(sa2, "xto_a", [128, 16, 1056], BF16)
                        S.dma('pool', [], ['xto'], out=xto[:], in_=xTo.rearrange("(c p) t -> p c t", p=128))
                        Wqh = [sb(sa2, "Wq%d" % i, [128, 16, 128], BF16) for i in range(2)]
                        qf = [sb(sa2, "qf%d" % i, [128, 512]) for i in range(2)]
                        dt_sb = sb(sa2, "dt_sb", [128, NT, 32])
                        fp_sb = sb(sa2, "fp_sb", [128, NT, 32])
                        gm = [sb(sa2, "gm%d" % i, [128, 32]) for i in range(2)]
                        t8 = [sb(sa2, "t8%d" % i, [128, 8]) for i in range(2)]
                        thr = [sb(sa2, "thr%d" % i, [128, 1]) for i in range(2)]
                        sel = [sb(sa2, "sel%d" % i, [128, 32]) for i in range(2)]
                        ex = [sb(sa2, "ex%d" % i, [128, 32]) for i in range(2)]
                        S.dma('sp', [], ['dt_sb'], out=dt_sb[:], in_=dtab.rearrange("p (i n) -> p i n", n=32))
                        S.dma('sp', [], ['fp_sb'], out=fp_sb[:], in_=fptab.rearrange("p (i n) -> p i n", n=32))
                        S.op('pool', 'memset', [], [('Vo1',)], Vo[:, :, :, 128:129], 1.0)
                        cnt = 0
                        for h in range(NH):
                            S.dma('pool', [], [('Wq', h % 2)], out=Wqh[h % 2][:], in_=w_in_v[:, :, h * 128:(h + 1) * 128])
                            for qc in range(2):
                                pb = qc
                                for c in range(16):
                                    S.op('pe', 'matmul', [('Wq', h % 2), 'xto'], [('ps', pb)], PS[pb][:],
                                         lhsT=Wqh[h % 2][:, c, :], rhs=xto[:, c, 32 + qc * 512:32 + (qc + 1) * 512],
                                         start=(c == 0), stop=(c == 15))
                                S.op('dve', 'tensor_copy', [('ps', pb)], [('qf', qc)], out=qf[qc][:], in_=PS[pb][:])
                                S.op('act', 'activation', [('qf', qc)], [('QT', h, qc)], out=QT[:, h, qc * 512:(qc + 1) * 512], in_=qf[qc][:], func=AF.Copy)
                                for j in range(4):
                                    i = qc * 4 + j
                                    k2 = cnt % 2
                                    cnt += 1
                                    gp = PS[4 + k2][:, 0:32]
                                    S.op('pe', 'matmul', [('qf', qc), 'kmean'], [('ps', 4 + k2)], gp,
                                         lhsT=qf[qc][:, j * 128:(j + 1) * 128], rhs=kmean[:, h, :], start=True, stop=True)
                                    S.op('dve', 'tensor_tensor', [('ps', 4 + k2), 'fp_sb'], [('gm', k2)], out=gm[k2][:], in0=gp, in1=fp_sb[:, i, :], op=ALU.add)
                                    S.op('dve', 'max', [('gm', k2)], [('t8', k2)], out=t8[k2][:], in_=gm[k2][:])
                                    S.op('dve', 'tensor_scalar', [('t8', k2)], [('thr', k2)], out=thr[k2][:], in0=t8[k2][:, 2:3], scalar1=-1.0e29, scalar2=None, op0=ALU.max)
                                    S.op('dve', 'tensor_scalar', [('gm', k2), ('thr', k2)], [('sel', k2)], out=sel[k2][:], in0=gm[k2][:], scalar1=thr[k2][:, 0:1], scalar2=None, op0=ALU.is_ge)
                                    S.op('act', 'activation', ['dt_sb'], [('ex', k2)], out=ex[k2][:], in_=dt_sb[:, i, :], func=AF.Exp, scale=SLOPES[h])
                                    S.op('dve', 'tensor_tensor', [('sel', k2), ('ex', k2)], [('Wt', h, i)], out=Wt[:, h, i, :], in0=sel[k2][:], in1=ex[k2][:], op=ALU.mult)
                            for qc in range(2):
                                pb = 2 + qc
                                for c in range(16):
                                    S.op('pe', 'matmul', ['Wk', 'xto'], [('ps', pb)], PS[pb][:],
                                         lhsT=Wk[:, c, h * 128:(h + 1) * 128], rhs=xto[:, c, 32 + qc * 512:32 + (qc + 1) * 512],
                                         start=(c == 0), stop=(c == 15))
                                S.op('act', 'activation', [('ps', pb)], [('KTo', h, qc)], out=KTo[:, h, qc * 512:(qc + 1) * 512], in_=PS[pb][:], func=AF.Copy)
                        for i in range(NT):
                            for half in range(2):
                                pb = 6 + half
                                for c in range(16):
                                    S.op('pe', 'matmul', ['Wv', 'xto'], [('ps', pb)], PS[pb][:],
                                         lhsT=xto[:, c, 32 + i * 128:32 + (i + 1) * 128], rhs=Wv[:, c, half * 512:(half + 1) * 512],
                                         start=(c == 0), stop=(c == 15))
                                S.op('dve' if half else 'act', 'tensor_copy' if half else 'activation', [('ps', pb)], [('Vo', i, half)],
                                     **(dict(out=Vo[:, i, half * 4:(half + 1) * 4, 0:128], in_=PS[pb][:].rearrange("p (h d) -> p h d", d=128))
                                        if half else dict(out=Vo[:, i, 0:4, 0:128], in_=PS[pb][:].rearrange("p (h d) -> p h d", d=128), func=AF.Copy)))
                        S.barrier()
                        if stop == 3:
                            S.dma('sp', [], ['dbgb3'], out=dbgb[:, 0:8192], in_=QT[:].rearrange('p a d -> p (a d)'))
                            S.dma('sp', [], ['dbgb3b'], out=dbgb[:, 8192:16384], in_=KTo[:].rearrange('p a d -> p (a d)'))
                            dump_f(Wt[:].rearrange('p a b c -> p (a b c)'), 0, 2048)
                            dump_f(kmean[:].rearrange('p a b -> p (a b)'), 2048, 256)
                            raise _Stop()

                attnT = sb(s1, "attnT", [128, NH, TOK], BF16)
                with ExitStack() as sbk:
                    KTh = [accb[:, i * 8192:(i + 1) * 8192] for i in range(2)]
                    Vh = [accb[:, 16384:16384 + 64 * 129].rearrange("p (n d) -> p n d", d=129), sb(sbk, "Vh1", [128, 64, 129], BF16)]
                    PT = [sb(sbk, "PT%d" % i, [128, 512], BF16) for i in range(4)]
                    PO = [sb(sbk, "PO%d" % i, [128, 128], BF16) for i in range(2)]
                    ac = sb(sbk, "ac", [128, NT, 129])
                    wown = sb(sbk, "wown", [128, NT])
                    rec = sb(sbk, "rec", [128, NT])
                    atok = [sb(sbk, "atok%d" % i, [128, 128], BF16) for i in range(2)]
                    Vs_v = Vs.rearrange("(n p) d -> p n d", p=128)
                    for i in range(2):
                        S.op('pool', 'memset', [], [('Vh1', i)], Vh[i][:, :, 128:129], 1.0)
                    pti = 0
                    oi = 0
                    for h in range(NH):
                        hb = h % 2
                        S.dma('sp', [], [('KTh', hb)], out=KTh[hb][:], in_=KTs[h, :, :])
                        for vg in range(8):
                            S.dma('sp', [('Vh1', hb)], [('Vh', hb, vg)], out=Vh[hb][:, vg * 8:(vg + 1) * 8, 0:128], in_=Vs_v[:, vg * 8:(vg + 1) * 8, h * 128:(h + 1) * 128])
                        S.op('dve', 'memset', [], [('ac', i) for i in range(NT)], ac[:], 0.0)
                        S.op('act', 'activation', ['c2'], ['wown'], out=wown[:], in_=down, func=AF.Exp, scale=SLOPES[h])
                        steps = [(n, qc) for n in range(31) for qc in range(2)]

                        def emit_st(n, qc):
                            nonlocal pti
                            pts = []
                            for kk in range(2):
                                pb = pti % 4
                                pti += 1
                                S.op('pe', 'matmul', [('KTh', hb), ('QT', h, qc)], [('ps', pb)], PS[pb][:],
                                     lhsT=KTh[hb][:, (2 * n + kk) * 128:(2 * n + kk + 1) * 128], rhs=QT[:, h, qc * 512:(qc + 1) * 512], start=True, stop=True)
                                pts.append(pb)
                            return pts
                        nxt = emit_st(*steps[0])
                        for si, (n, qc) in enumerate(steps):
                            pts = nxt
                            for kk in range(2):
                                pb = pts[kk]
                                S.op('act', 'activation', [('ps', pb), 'c2'], [('PT', pb)], out=PT[pb][:], in_=PS[pb][:], func=AF.Exp,
                                     scale=SCALE, bias=kbias[:, 2 * h + kk:2 * h + kk + 1])
                            if si + 1 < len(steps):
                                nxt = emit_st(*steps[si + 1])
                            for qs in range(4):
                                i = qc * 4 + qs
                                ob = oi % 4
                                oi += 1
                                ops_ = PS[4 + ob][:, 0:129]
                                for kk in range(2):
                                    S.op('pe', 'matmul', [('PT', pts[kk]), ('Vh', hb, (2 * n + kk) // 8)], [('po', ob)], ops_,
                                         lhsT=PT[pts[kk]][:, qs * 128:(qs + 1) * 128], rhs=Vh[hb][:, 2 * n + kk, :], start=(kk == 0), stop=(kk == 1))
                                S.op('dve', 'scalar_tensor_tensor', [('po', ob), ('Wt', h, i), ('ac', i)], [('ac', i)], out=ac[:, i, :], in0=ops_,
                                     scalar=Wt[:, h, i, n:n + 1], in1=ac[:, i, :], op0=ALU.mult, op1=ALU.add)
                        for i in range(NT):
                            ob = oi % 4
                            oi += 1
                            ops_ = PS[4 + ob][:, 0:129]
                            nk = i % 2 + 1
                            for kk in range(nk):
                                pb = pti % 4
                                pti += 1
                                k0 = (i // 2) * 256 + kk * 128
                                S.op('pe', 'matmul', [('KTo', h, k0 // 512), ('QT', h, i // 4)], [('ps', pb)], PS[pb][:, 0:128],
                                     lhsT=KTo[:, h, k0:k0 + 128], rhs=QT[:, h, i * 128:(i + 1) * 128], start=True, stop=True)
                                po = (pti) % 2
                                S.op('act', 'activation', [('ps', pb), 'c2'], [('PO', po)], out=PO[po][:], in_=PS[pb][:, 0:128], func=AF.Exp,
                                     scale=SCALE, bias=kbias[:, 2 * h + kk:2 * h + kk + 1])
                                if kk == i % 2:
                                    S.op('dve', 'tensor_tensor', [('PO', po), 'cb'], [('PO', po)], out=PO[po][:], in0=PO[po][:], in1=trib, op=ALU.mult)
                                vt = (i // 2) * 2 + kk
                                S.op('pe', 'matmul', [('PO', po), ('Vo', vt, h // 4), ('Vo1',)], [('po', ob)], ops_,
                                     lhsT=PO[po][:], rhs=Vo[:, vt, h, :], start=(kk == 0), stop=(kk == nk - 1))
                            S.op('dve', 'scalar_tensor_tensor', [('po', ob), 'wown', ('ac', i)], [('ac', i)], out=ac[:, i, :], in0=ops_,
                                 scalar=wown[:, i:i + 1], in1=ac[:, i, :], op0=ALU.mult, op1=ALU.add)
                        S.op('dve', 'reciprocal', [('ac', i) for i in range(NT)], ['rec'], out=rec[:], in_=ac[:, :, 128])
                        for i in range(NT):
                            ab = i % 2
                            S.op('dve', 'tensor_scalar', [('ac', i), 'rec'], [('atok', ab)], out=atok[ab][:], in0=ac[:, i, 0:128], scalar1=rec[:, i:i + 1], scalar2=None, op0=ALU.mult)
                            pb = pti % 4
                            pti += 1
                            S.op('pe', 'matmul', [('atok', ab), 'cb'], [('ps', pb)], PS[pb][:, 0:128], lhsT=atok[ab][:], rhs=identb, start=True, stop=True)
                            S.op('act', 'activation', [('ps', pb)], [('attnT', h, i)], out=attnT[:, h, i * 128:(i + 1) * 128], in_=PS[pb][:, 0:128], func=AF.Copy)
                    S.barrier()
                    if stop == 4:
                        S.dma('sp', [], ['dbgb4'], out=dbgb[:, 0:8192], in_=attnT[:].rearrange('p a d -> p (a d)'))
                        raise _Stop()

                with ExitStack() as sc3:
                    wo = [sb(sc3, "wo%d" % i, [128, 16, 512], BF16) for i in range(2)]
                    xr = [sb(sc3, "xr%d" % i, [128, 512]) for i in range(3)]
                    w_out_v = w_out.rearrange("(c p) f -> p c f", p=128)
                    xi = 0
                    for cc in range(4):
                        b = cc % 2
                        S.dma('pool', [], [('wo', b)], out=wo[b][:], in_=w_out_v[:, :, cc * 512:(cc + 1) * 512])
                        for i in range(NT):
                            pb = i % 4
                            xb = xi % 3
                            xi += 1
                            S.dma('sp', [], [('xr', xb)], out=xr[xb][:], in_=xo[i * 128:(i + 1) * 128, cc * 512:(cc + 1) * 512])
                            for c in range(16):
                                src = attnT if c < 8 else convT
                                S.op('pe', 'matmul', [('wo', b)], [('ps', pb)], PS[pb][:],
                                     lhsT=src[:, c % 8, i * 128:(i + 1) * 128], rhs=wo[b][:, c, :], start=(c == 0), stop=(c == 15))
                            S.op('dve', 'scalar_tensor_tensor', [('ps', pb), ('xr', xb)], [('acc', i, cc)], out=acc[:, i, cc * 512:(cc + 1) * 512], in0=xr[xb][:],
                                 scalar=ALPHA, in1=PS[pb][:], op0=ALU.mult, op1=ALU.add)
                    S.barrier()
                    if stop == 5:
                        dump_f(accraw[:].bitcast(F32), 0, 16384)
                        raise _Stop()

            def layer_norm_tile(src_ap, dst_ap, gb, stats, mv, rs, tmp, keys_r, keys_w, tag):
                for k in range(4):
                    S.op('dve', 'bn_stats', keys_r, [(tag, 'st', k)], out=stats[:, k, :], in_=src_ap[:, k * 512:(k + 1) * 512])
                S.op('dve', 'bn_aggr', [(tag, 'st', k) for k in range(4)], [(tag, 'mv')], out=mv[:], in_=stats[:].rearrange("p a b -> p (a b)"))
                S.op('dve', 'tensor_scalar', [(tag, 'mv')], [(tag, 'rs')], out=rs[:], in0=mv[:, 1:2], scalar1=EPS, scalar2=None, op0=ALU.add)
                S.op('act', 'activation', [(tag, 'rs')], [(tag, 'rs')], out=rs[:], in_=rs[:], func=AF.Sqrt)
                S.op('dve', 'reciprocal', [(tag, 'rs')], [(tag, 'rs')], out=rs[:], in_=rs[:])
                S.op('dve', 'tensor_scalar', keys_r + [(tag, 'mv'), (tag, 'rs')], [(tag, 'tmp')], out=tmp[:], in0=src_ap, scalar1=mv[:, 0:1], scalar2=rs[:, 0:1],
                     op0=ALU.subtract, op1=ALU.mult)
                S.op('dve', 'tensor_tensor', [(tag, 'tmp'), tag + 'gb'], [(tag, 'tmp')], out=tmp[:], in0=tmp[:], in1=gb[:, 0, :], op=ALU.mult)
                S.op('dve', 'tensor_tensor', [(tag, 'tmp'), tag + 'gb'], keys_w, out=dst_ap, in0=tmp[:], in1=gb[:, 1, :], op=ALU.add)

            with ExitStack() as s2:
                h1b = sb(s2, "h1b", [128, NT, DM], BF16)
                gate = sb(s2, "gate", [128, NT, NE])
                rankm = sb(s2, "rankm", [128, NT, NE])
                gateT = sb(s2, "gateT", [NE, TOK])
                bgs = sb(s2, "bgs", [128, NE * 32])
                S.dma('sp', [], ['bgs'], out=bgs[:], in_=bgu[:, :])
                with ExitStack() as sr:
                    gb1 = sb(sr, "gb1", [128, 2, DM])
                    stats = sb(sr, "stats", [128, 4, 6])
                    mv = sb(sr, "mv", [128, 2])
                    rs = sb(sr, "rs", [128, 1])
                    tmp = sb(sr, "tmp", [128, DM])
                    h1f = [sb(sr, "h1f%d" % i, [128, DM]) for i in range(2)]
                    hT = [sb(sr, "hT%d" % i, [128, 128]) for i in range(4)]
                    wr = sb(sr, "wr", [128, 16, NE])
                    brb = sb(sr, "brb", [128, NE])
                    lg = sb(sr, "lg", [128, NE])
                    t8 = sb(sr, "t8r", [128, 8])
                    nmx = sb(sr, "nmx", [128, 1])
                    selt = sb(sr, "selt", [128, NT, NE])
                    selb = sb(sr, "selb", [128, NT, NE], BF16)
                    exr = sb(sr, "exr", [128, NE])
                    den = sb(sr, "den", [128, 1])
                    rkT = sb(sr, "rkT", [NE, TOK])
                    slT = sb(sr, "slT", [NE, TOK])
                    bds = sb(sr, "bds", [NE, DM])
                    tr = sb(sr, "tr", [128, NE])
                    S.dma('sp', [], ['ln1gb'], out=gb1[:].rearrange("p a d -> p (a d)"), in_=ln1.rearrange("a d -> (a d)").partition_broadcast(128))
                    S.dma('sp', [], ['wr'], out=wr[:], in_=w_r.rearrange("(c p) e -> p c e", p=128))
                    S.dma('sp', [], ['brb'], out=brb[:], in_=b_r[0, :].partition_broadcast(128))
                    S.dma('sp', [], ['bds'], out=bds[:], in_=b_dn[:, :])
                    hti = 0
                    for i in range(NT):
                        fb = i % 2
                        layer_norm_tile(acc[:, i, :], h1f[fb][:], gb1, stats, mv, rs, tmp, [('acc', i)], [('h1f', fb)], 'ln1')
                        S.op('act', 'activation', [('h1f', fb)], [('h1b', i)], out=h1b[:, i, :], in_=h1f[fb][:], func=AF.Copy)
                        S.op('act', 'activation', [('h1f', fb)], [('acc', i)], out=acc[:, i, :], in_=h1f[fb][:], func=AF.Copy, scale=ALPHA)
                        for c in range(16):
                            pb = c % 4
                            S.op('pe', 'matmul', [('h1f', fb), 'cf'], [('ps', pb)], PS[pb][:, 0:128], lhsT=h1f[fb][:, c * 128:(c + 1) * 128], rhs=identf, start=True, stop=True)
                            tb = hti % 4
                            hti += 1
                            S.op('act' if c % 2 else 'dve', 'activation' if c % 2 else 'tensor_copy', [('ps', pb)], [('hT', tb)],
                                 **(dict(out=hT[tb][:], in_=PS[pb][:, 0:128], func=AF.Copy) if c % 2 else dict(out=hT[tb][:], in_=PS[pb][:, 0:128])))
                            S.op('pe', 'matmul', [('hT', tb), 'wr'], [('ps', 4)], PS[4][:, 0:NE], lhsT=hT[tb][:], rhs=wr[:, c, :], start=(c == 0), stop=(c == 15))
                        S.op('dve', 'tensor_tensor', [('ps', 4), 'brb'], ['lg'], out=lg[:], in0=PS[4][:, 0:NE], in1=brb[:], op=ALU.add)
                        S.op('dve', 'max', ['lg'], ['t8'], out=t8[:], in_=lg[:])
                        S.op('dve', 'tensor_scalar', ['lg', 't8'], [('selt', i)], out=selt[:, i, :], in0=lg[:], scalar1=t8[:, 3:4], scalar2=None, op0=ALU.is_ge)
                        S.op('dve', 'tensor_scalar', ['t8'], ['nmx'], out=nmx[:], in0=t8[:, 0:1], scalar1=-1.0, scalar2=None, op0=ALU.mult)
                        S.op('act', 'activation', ['lg', 'nmx'], ['exr'], out=exr[:], in_=lg[:], func=AF.Exp, bias=nmx[:, 0:1], scale=1.0)
                        S.op('dve', 'tensor_tensor', ['exr', ('selt', i)], ['exr'], out=exr[:], in0=exr[:], in1=selt[:, i, :], op=ALU.mult)
                        S.op('dve', 'tensor_reduce', ['exr'], ['den'], out=den[:], in_=exr[:], axis=AX.X, op=ALU.add)
                        S.op('dve', 'reciprocal', ['den'], ['den'], out=den[:], in_=den[:])
                        S.op('dve', 'tensor_scalar', ['exr', 'den'], [('gate', i)], out=gate[:, i, :], in0=exr[:], scalar1=den[:, 0:1], scalar2=None, op0=ALU.mult)
                        S.op('dve', 'tensor_copy', [('selt', i)], [('selb', i)], out=selb[:, i, :], in_=selt[:, i, :])
                    for i in range(NT):
                        for i2 in range(i + 1):
                            S.op('pe', 'matmul', [('selb', i2), 'cb'], [('ps', 5)], PS[5][:, 0:NE], lhsT=(utrib if i2 == i else onesb), rhs=selb[:, i2, :], start=(i2 == 0), stop=(i2 == i))
                        S.op('dve', 'tensor_tensor', [('ps', 5), ('selt', i)], ['tr'], out=tr[:], in0=PS[5][:, 0:NE], in1=selt[:, i, :], op=ALU.mult)
                        S.op('dve', 'scalar_tensor_tensor', ['tr', ('selt', i)], [('rankm', i)], out=rankm[:, i, :], in0=selt[:, i, :], scalar=-1.0, in1=tr[:], op0=ALU.add, op1=ALU.add)
                        for i2 in range(i + 1):
                            S.op('pe', 'matmul', [('selb', i2), 'cb'], [('ps', 6)], PS[6][0:NE, 0:128], lhsT=selb[:, i2, :], rhs=(utrib if i2 == i else onesb), start=(i2 == 0), stop=(i2 == i))
                        S.op('pe', 'matmul', [('selb', i), 'cb'], [('ps', 7)], PS[7][0:NE, 0:128], lhsT=selb[:, i, :], rhs=identb, start=True, stop=True)
                        S.op('pe', 'matmul', [('gate', i), 'cf'], [('ps', 3)], PS[3][0:NE, 0:128], lhsT=gate[:, i, :], rhs=identf, start=True, stop=True)
                        ts_ = slice(i * 128, (i + 1) * 128)
                        S.op('act', 'activation', [('ps', 7)], [('slT', i)], out=slT[:, ts_], in_=PS[7][0:NE, 0:128], func=AF.Copy)
                        S.op('act', 'activation', [('ps', 3)], [('gateT', i)], out=gateT[:, ts_], in_=PS[3][0:NE, 0:128], func=AF.Copy)
                        S.op('dve', 'tensor_tensor', [('ps', 6), ('slT', i)], [('rkT', i)], out=rkT[:, ts_], in0=PS[6][0:NE, 0:128], in1=slT[:, ts_], op=ALU.mult)
                        S.op('dve', 'scalar_tensor_tensor', [('rkT', i), ('slT', i)], [('rkT', i)], out=rkT[:, ts_], in0=slT[:, ts_], scalar=-1.0, in1=rkT[:, ts_], op0=ALU.add, op1=ALU.add)
                    S.dma('sp', [('rkT', i) for i in range(NT)], ['RK'], out=RK[:, :], in_=rkT[:])
                    S.dma('sp', [('gateT', i) for i in range(NT)], ['GT'], out=GT[:, :], in_=gateT[:])
                    k = 0
                    for i in range(NT):
                        for cc in range(4):
                            pb = k % 4
                            k += 1
                            S.op('pe', 'matmul', [('gateT', i), 'bds'], [('ps', pb)], PS[pb][:], lhsT=gateT[:, i * 128:(i + 1) * 128], rhs=bds[:, cc * 512:(cc + 1) * 512], start=True, stop=True)
                            S.op('dve', 'tensor_tensor', [('ps', pb), ('acc', i)], [('acc', i)], out=acc[:, i, cc * 512:(cc + 1) * 512], in0=PS[pb][:], in1=acc[:, i, cc * 512:(cc + 1) * 512], op=ALU.add)
                    S.barrier()
                    if stop == 6:
                        dump_f(accraw[:].bitcast(F32), 0, 16384)
                        S.dma('sp', [], ['dbgb6'], out=dbgb[:, 0:16384], in_=h1b[:].rearrange('p a d -> p (a d)'))
                        raise _Stop()

                with ExitStack() as sd:
                    wgu = [sb(sd, "wgu%d" % i, [128, 16, 512], BF16) for i in range(2)]
                    wdn = [sb(sd, "wdn%d" % i, [128, 16, 256], BF16) for i in range(2)]
                    xg = sb(sd, "xg", [128, 16, CAP], BF16)
                    actT = sb(sd, "actT", [128, 16, CAP], BF16)
                    selE = [sb(sd, "selE%d" % i, [128, NT, CAP], BF16) for i in range(1)]
                    selG = [sb(sd, "selG%d" % i, [128, 2, TOK], BF16) for i in range(1)]
                    rbc = [sb(sd, "rbc%d" % i, [128, TOK]) for i in range(1)]
                    gbc = [sb(sd, "gbc%d" % i, [128, TOK]) for i in range(1)]
                    yb = [sb(sd, "yb%d" % i, [128, 2, 256], BF16) for i in range(2)]
                    g1 = [sb(sd, "g1%d" % i, [128, CAP]) for i in range(2)]
                    sgm = [sb(sd, "sgm%d" % i, [128, CAP]) for i in range(2)]
                    u1 = [sb(sd, "u1%d" % i, [128, CAP]) for i in range(2)]
                    gui = 0
                    dni = 0
                    xgi = 0
                    pri = 0
                    pend = None
                    for e in range(NE):
                        eb = 0
                        S.dma('sp', ['RK'], [('rbc', eb)], out=rbc[eb][:], in_=RK[e, :].partition_broadcast(128))
                        S.dma('sp', ['GT'], [('gbc', eb)], out=gbc[eb][:], in_=GT[e, :].partition_broadcast(128))
                        for i in range(NT):
                            S.op('dve', 'tensor_scalar', ['cf', ('rankm', i)], [('selE', eb, i)], out=selE[eb][:, i, :], in0=iota_row[:, 0:CAP], scalar1=rankm[:, i, e:e + 1], scalar2=None, op0=ALU.is_equal)
                        for j in range(2):
                            mj = MJ[j]
                            S.op('dve', 'scalar_tensor_tensor', [('rbc', eb), ('gbc', eb), 'c2'], [('selG', eb, j)], out=selG[eb][0:mj, j, :], in0=rbc[eb][0:mj, :],
                                 scalar=iota_p2[0:mj, j:j + 1], in1=gbc[eb][0:mj, :], op0=ALU.is_equal, op1=ALU.mult)
                        for c in range(16):
                            hbk = xgi % 2
                            xgi += 1
                            gp = PS[hbk][:, 0:CAP]
                            for i in range(NT):
                                S.op('pe', 'matmul', [('h1b', i), ('selE', eb, i)], [('psg', hbk)], gp, lhsT=h1b[:, i, c * 128:(c + 1) * 128], rhs=selE[eb][:, i, :], start=(i == 0), stop=(i == NT - 1))
                            S.op('act', 'activation', [('psg', hbk)], [('xg', c)], out=xg[:, c, :], in_=gp, func=AF.Copy)
                        xgk = [('xg', c) for c in range(16)]
                        w_gu_v = w_gu[e].rearrange("(c p) f -> p c f", p=128)
                        for q in range(8):
                            gb_ = gui % 2
                            gui += 1
                            S.dma('pool', [], [('wgu', gb_, 0)], out=wgu[gb_][:, :, 0:256], in_=w_gu_v[:, :, q * 256:(q + 1) * 256])
                            S.dma('pool', [], [('wgu', gb_, 1)], out=wgu[gb_][:, :, 256:512], in_=w_gu_v[:, :, DM + q * 256:DM + (q + 1) * 256])
                            for j in range(2):
                                fc = q * 2 + j
                                pb = 2 + pri % 2
                                pri += 1
                                tb = pri % 2
                                gps = PS[pb][:, 0:CAP]
                                ups = PS[pb][:, 256:256 + CAP]
                                for c in range(16):
                                    S.op('pe', 'matmul', [('wgu', gb_, 0)] + xgk, [('psgu', pb, 0)], gps, lhsT=wgu[gb_][:, c, j * 128:(j + 1) * 128], rhs=xg[:, c, :], start=(c == 0), stop=(c == 15))
                                for c in range(16):
                                    S.op('pe', 'matmul', [('wgu', gb_, 1)] + xgk, [('psgu', pb, 1)], ups, lhsT=wgu[gb_][:, c, 256 + j * 128:256 + (j + 1) * 128], rhs=xg[:, c, :], start=(c == 0), stop=(c == 15))
                                bg = bgs[:, e * 32 + fc:e * 32 + fc + 1]
                                bu = bgs[:, e * 32 + 16 + fc:e * 32 + 16 + fc + 1]
                                S.op('dve', 'tensor_scalar', [('psgu', pb, 0), ('psgu', pb, 1), 'bgs'], [('g1', tb)], out=g1[tb][:], in0=gps, scalar1=bg, scalar2=7.0, op0=ALU.add, op1=ALU.min)
                                S.op('act', 'activation', [('g1', tb)], [('sgm', tb)], out=sgm[tb][:], in_=g1[tb][:], func=AF.Sigmoid, scale=1.702)
                                S.op('dve', 'tensor_scalar', [('psgu', pb, 1), 'bgs'], [('u1', tb)], out=u1[tb][:], in0=ups, scalar1=bu, scalar2=7.0, op0=ALU.add, op1=ALU.min)
                                S.op('dve', 'tensor_scalar', [('u1', tb)], [('u1', tb)], out=u1[tb][:], in0=u1[tb][:], scalar1=-7.0, scalar2=1.0, op0=ALU.max, op1=ALU.add)
                                if pend is not None:
                                    ptb, pfc = pend
                                    S.op('dve', 'tensor_tensor', [('g1', ptb), ('sgm', ptb)], [('g1', ptb)], out=g1[ptb][:], in0=g1[ptb][:], in1=sgm[ptb][:], op=ALU.mult)
                                    S.op('dve', 'tensor_tensor', [('g1', ptb), ('u1', ptb)], [('actT', pfc)], out=actT[:, pfc, :], in0=g1[ptb][:], in1=u1[ptb][:], op=ALU.mult)
                                pend = (tb, fc)
                        ptb, pfc = pend
                        S.op('dve', 'tensor_tensor', [('g1', ptb), ('sgm', ptb)], [('g1', ptb)], out=g1[ptb][:], in0=g1[ptb][:], in1=sgm[ptb][:], op=ALU.mult)
                        S.op('dve', 'tensor_tensor', [('g1', ptb), ('u1', ptb)], [('actT', pfc)], out=actT[:, pfc, :], in0=g1[ptb][:], in1=u1[ptb][:], op=ALU.mult)
                        pend = None
                        ak = [('actT', fc) for fc in range(16)]
                        w_dn_v = w_dn[e].rearrange("(c p) f -> p c f", p=128)
                        for r in range(8):
                            db = dni % 2
                            dni += 1
                            S.dma('pool', [], [('wdn', db)], out=wdn[db][:], in_=w_dn_v[:, :, r * 256:(r + 1) * 256])
                            for j in range(2):
                                mj = MJ[j]
                                yp = PS[4 + j][0:mj, 0:256]
                                for c in range(16):
                                    S.op('pe', 'matmul', [('wdn', db)] + ak, [('psy', j)], yp, lhsT=actT[:, c, j * 128:j * 128 + mj], rhs=wdn[db][:, c, :], start=(c == 0), stop=(c == 15))
                                S.op('act', 'activation', [('psy', j)], [('yb', db, j)], out=yb[db][0:mj, j, :], in_=yp, func=AF.Copy)
                            for i in range(NT):
                                sp_ = PS[6 + i % 2][:, 0:256]
                                for j in range(2):
                                    S.op('pe', 'matmul', [('selG', eb, j), ('yb', db, j)], [('pss', i % 2)], sp_, lhsT=selG[eb][0:MJ[j], j, i * 128:(i + 1) * 128], rhs=yb[db][0:MJ[j], j, :], start=(j == 0), stop=(j == 1))
                                S.op('dve', 'tensor_tensor', [('pss', i % 2), ('acc', i, r)], [('acc', i, r)], out=acc[:, i, r * 256:(r + 1) * 256], in0=sp_, in1=acc[:, i, r * 256:(r + 1) * 256], op=ALU.add)
                    S.barrier()
                    if stop == 7:
                        dump_f(accraw[:].bitcast(F32), 0, 16384)
                        raise _Stop()

                with ExitStack() as se:
                    gb2 = sb(se, "gb2", [128, 2, DM])
                    stats = sb(se, "stats2", [128, 4, 6])
                    mv = sb(se, "mv2", [128, 2])
                    rs = sb(se, "rs2", [128, 1])
                    tmp = sb(se, "tmp2", [128, DM])
                    ob_ = [sb(se, "ob%d" % i, [128, DM]) for i in range(2)]
                    S.dma('sp', [], ['ln2gb'], out=gb2[:].rearrange("p a d -> p (a d)"), in_=ln2.rearrange("a d -> (a d)").partition_broadcast(128))
                    for i in range(NT):
                        fb = i % 2
                        layer_norm_tile(acc[:, i, :], ob_[fb][:], gb2, stats, mv, rs, tmp, [('acc', i)], [('ob', fb)], 'ln2')
                        S.dma('sp', [('ob', fb)], [('out', i)], out=out[i * 128:(i + 1) * 128, :], in_=ob_[fb][:])
        except _Stop:
            pass
        S.finalize(st)
    return nc


_CACHE = {}


def _consts():
    p = np.arange(128)
    cst = np.zeros((128, 1024), np.float32)
    cst[:, 0:128] = np.eye(128)
    cst[:, 128:256] = (p[:, None] <= p[None, :])
    cst[:, 256:384] = (p[:, None] < p[None, :])
    cst[:, 384:512] = 1.0
    cst[:, 512:768] = np.arange(256)[None, :]
    c2 = np.zeros((128, 64), np.float32)
    c2[:, 0] = p
    c2[:, 1] = p + 128
    for h in range(NH):
        for kk in range(2):
            c2[:, 2 + 2 * h + kk] = SLOPES[h] * (p + 128 * kk - 128)
    for i in range(NT):
        c2[:, 18 + i] = 128 - (i % 2) * 128 - p
    return cst, c2


def kernel(x, w_in, conv_w, conv_b, conv_ln_g, conv_ln_b, w_out, ln1_g, ln1_b,
           w_router, b_router, w_gate_up, b_gate_up, w_down, b_down, ln2_g, ln2_b):
    f = lambda a: np.ascontiguousarray(np.asarray(a, dtype=np.float32))
    x2 = f(x)[0]
    xT = np.ascontiguousarray(x2.T)
    if 'nc' not in _CACHE:
        _CACHE['nc'] = build()
    nc = _CACHE['nc']
    cst, c2 = _consts()
    convw = np.ascontiguousarray(f(conv_w).T.reshape(8, 128, 31).transpose(1, 0, 2).reshape(128, 8 * 31))
    cv = np.concatenate([f(v).reshape(8, 128).T for v in (conv_b, conv_ln_g, conv_ln_b)], axis=1)
    bgu = np.ascontiguousarray(f(b_gate_up).reshape(NE, 32, 128).transpose(2, 0, 1).reshape(128, NE * 32))
    shared = dict(
        xT=xT, w_in=f(w_in), convw=convw, convv=np.ascontiguousarray(cv), w_out=f(w_out),
        ln1=np.stack([f(ln1_g), f(ln1_b)]), ln2=np.stack([f(ln2_g), f(ln2_b)]),
        w_r=f(w_router), b_r=f(b_router).reshape(1, NE), w_gu=f(w_gate_up), bgu=bgu,
        w_dn=f(w_down), b_dn=f(b_down), cst=cst, cst2=c2)
    in_maps = []
    p = np.arange(128)
    for c in range(NCORES):
        t0 = c * TOK
        xto = np.zeros((DM, 1056), np.float32)
        lo = max(t0 - 32, 0)
        xto[:, 1056 - (t0 + TOK - lo):] = xT[:, lo:t0 + TOK]
        tq = t0 + np.arange(NT)[None, :, None] * 128 + p[:, None, None]
        n = np.arange(32)[None, None, :]
        past = n < (tq // 256)
        dt = np.where(past, -(tq - 256 * n - 128), 0).astype(np.float32).reshape(128, 256)
        fp = np.where(past, 0.0, NEG).astype(np.float32).reshape(128, 256)
        m = dict(shared)
        m.update(xTo=xto, xo=np.ascontiguousarray(x2[t0:t0 + TOK]), dtab=np.ascontiguousarray(dt), fptab=np.ascontiguousarray(fp))
        in_maps.append(m)
    res = run_bass_kernel_spmd(nc, in_maps, core_ids=list(range(NCORES)))
    outp = np.concatenate([r["out"] for r in res.results], axis=0)
    return outp.reshape(1, SEQ, DM).astype(np.float32)
```

```python
import os
import numpy as np
from contextlib import ExitStack
import concourse.bass as bass
import concourse.mybir as mybir
from concourse.bass_utils import run_bass_kernel_spmd

F32 = mybir.dt.float32
BF16 = mybir.dt.bfloat16
AF = mybir.ActivationFunctionType
ALU = mybir.AluOpType
AX = mybir.AxisListType

NCORES = 8
SEQ = 8192
DM = 2048
TOK = 1024
NT = 8
NH = 8
NE = 32
CAP = 192
MJ = (128, CAP - 128)
ALPHA = 2.0 ** 0.25
SCALE = 128.0 ** -0.5
SLOPES = [2.0 ** (-(h + 1)) for h in range(NH)]
EPS = 1e-5
NEG = -1.0e30


class _Stop(Exception):
    pass


class _NoExc:
    def __init__(self, cm):
        self.cm = cm

    def __enter__(self):
        return self.cm.__enter__()

    def __exit__(self, *a):
        self.cm.__exit__(None, None, None)
        return False


class Sched:
    ENGS = ('pe', 'act', 'dve', 'pool', 'sp')
    NS = 8

    def __init__(self, nc):
        self.nc = nc
        self.streams = {e: [] for e in self.ENGS}
        self.lastw = {}
        self.readers = {}
        self.dma_since = []

    def _add(self, eng, fn, r, w, dma):
        idx = len(self.streams[eng])
        node = (eng, idx)
        deps = set()
        for k in r:
            lw = self.lastw.get(k)
            if lw is not None:
                deps.add(lw)
        for k in w:
            lw = self.lastw.get(k)
            if lw is not None and (lw[0] != eng or self.streams[lw[0]][lw[1]]['dma'] or dma):
                deps.add(lw)
            for rd in self.readers.get(k, ()):
                if rd[0] != eng or self.streams[rd[0]][rd[1]]['dma'] or dma:
                    deps.add(rd)
        deps.discard(node)
        rec = dict(fn=fn, deps=deps, dma=dma, has_dep=False)
        self.streams[eng].append(rec)
        for d in deps:
            self.streams[d[0]][d[1]]['has_dep'] = True
        for k in w:
            self.lastw[k] = node
            self.readers[k] = []
        for k in r:
            lst = self.readers.setdefault(k, [])
            if not dma:
                for j in range(len(lst)):
                    if lst[j][0] == eng and not self.streams[eng][lst[j][1]]['dma']:
                        lst[j] = node
                        break
                else:
                    lst.append(node)
            else:
                lst.append(node)
        if dma:
            self.dma_since.append(node)
        return node

    def op(self, eng, meth, r, w, *a, **k):
        return self._add(eng, lambda e: getattr(e, meth)(*a, **k), tuple(r), tuple(w), False)

    def dma(self, q, r, w, **k):
        return self._add(q, lambda e: e.dma_start(**k), tuple(r), tuple(w), True)

    def barrier(self):
        deps = set(self.dma_since)
        for e in self.ENGS:
            st = self.streams[e]
            for i in range(len(st) - 1, -1, -1):
                if st[i]['fn'] is not None and not st[i]['dma']:
                    deps.add((e, i))
                    break
        for d in deps:
            self.streams[d[0]][d[1]]['has_dep'] = True
        for e in self.ENGS:
            self.streams[e].append(dict(fn=None, deps=set(deps), dma=False, has_dep=False))
        self.dma_since = []
        self.lastw = {}
        self.readers = {}

    def finalize(self, stack):
        nc = self.nc
        esem = {e: stack.enter_context(nc.semaphore("es_" + e)) for e in self.ENGS}
        dsem = {e: [stack.enter_context(nc.semaphore("ds_%s%d" % (e, i))) for i in range(self.NS)]
                for e in ('sp', 'act', 'pool')}
        ndma = {}
        for e in self.ENGS:
            tick = 0
            nd = 0
            for rec in self.streams[e]:
                if rec['fn'] is None:
                    continue
                if rec['dma']:
                    s = dsem[e][nd % self.NS]
                    rec['tok'] = (s, 16 * (nd // self.NS + 1))
                    rec['pre'] = (s, 16 * (nd // self.NS))
                    nd += 1
                elif rec['has_dep']:
                    tick += 1
                    rec['tok'] = (esem[e], tick)
            ndma[e] = nd
        block = stack.enter_context(nc.Block())
        streams = self.streams
        NS = self.NS

        def emit(e, engobj):
            waited = {}

            def wait(tok):
                s, v = tok
                if v <= 0:
                    return
                key = id(s)
                if waited.get(key, (None, 0))[1] >= v:
                    return
                waited[key] = (s, v)
                engobj.wait_ge(s, v)
            for rec in streams[e]:
                for d in sorted(rec['deps']):
                    if d[0] == e and not streams[d[0]][d[1]]['dma'] and rec['fn'] is None:
                        continue
                    wait(streams[d[0]][d[1]]['tok'])
                if rec['fn'] is None:
                    continue
                if rec['dma']:
                    wait(rec['pre'])
                    rec['fn'](engobj).then_inc(rec['tok'][0], 16)
                else:
                    ins = rec['fn'](engobj)
                    if rec['has_dep']:
                        ins.then_inc(rec['tok'][0], 1)
            if e == 'sp':
                for q in ('sp', 'act', 'pool'):
                    nd = ndma[q]
                    for i in range(min(nd, NS)):
                        cnt = (nd - 1 - i) // NS + 1
                        wait((dsem[q][i], 16 * cnt))

        @block.tensor
        def _(eng):
            emit('pe', eng)

        @block.scalar
        def _(eng):
            emit('act', eng)

        @block.vector
        def _(eng):
            emit('dve', eng)

        @block.gpsimd
        def _(eng):
            emit('pool', eng)

        @block.sync
        def _(eng):
            emit('sp', eng)


def build(stop=99):
    nc = bass.Bass("TRN2", target_bir_lowering=False)

    def din(name, shape, dt=F32):
        return nc.dram_tensor(name, list(shape), dt, kind="ExternalInput").ap()

    xT = din("xT", [DM, SEQ])
    xTo = din("xTo", [DM, 1056])
    xo = din("xo", [TOK, DM])
    w_in = din("w_in", [DM, 5120])
    convw = din("convw", [128, 8 * 31])
    convv = din("convv", [128, 24])
    w_out = din("w_out", [DM, DM])
    ln1 = din("ln1", [2, DM])
    ln2 = din("ln2", [2, DM])
    w_r = din("w_r", [DM, NE])
    b_r = din("b_r", [1, NE])
    w_gu = din("w_gu", [NE, DM, 2 * DM]) if stop >= 7 else None
    bgu = din("bgu", [128, NE * 32])
    w_dn = din("w_dn", [NE, DM, DM]) if stop >= 7 else None
    b_dn = din("b_dn", [NE, DM])
    cst = din("cst", [128, 1024])
    cst2 = din("cst2", [128, 64])
    dtab = din("dtab", [128, 8 * 32])
    fptab = din("fptab", [128, 8 * 32])
    out = nc.dram_tensor("out", [TOK, DM], F32, kind="ExternalOutput").ap()
    if stop < 99:
        dbgf = nc.dram_tensor("dbgf", [128, 16384], F32, kind="ExternalOutput").ap()
        dbgb = nc.dram_tensor("dbgb", [128, 16384], BF16, kind="ExternalOutput").ap()
    KTs = nc.dram_tensor("KTs", [NH, 128, SEQ], BF16, kind="Internal").ap()
    Vs = nc.dram_tensor("Vs", [SEQ, 1024], BF16, kind="Internal").ap()
    RK = nc.dram_tensor("RK", [NE, TOK], F32, kind="Internal").ap()
    GT = nc.dram_tensor("GT", [NE, TOK], F32, kind="Internal").ap()

    with ExitStack() as st:
        S = Sched(nc)

        def dump_f(ap, off, n):
            S.dma('sp', [], [('dbgf', off)], out=dbgf[:, off:off + n], in_=ap)

        def sb(stack, name, shape, dt=F32):
            return stack.enter_context(_NoExc(nc.sbuf_tensor(name, list(shape), dt)))

        PS = [st.enter_context(nc.psum_tensor("ps%d" % i, [128, 512], F32)) for i in range(8)]

        cf = sb(st, "cf", [128, 1024])
        cb = sb(st, "cb", [128, 512], BF16)
        c2 = sb(st, "c2", [128, 64])
        S.dma('sp', [], ['cf'], out=cf[:], in_=cst[:, :])
        S.dma('sp', [], ['c2'], out=c2[:], in_=cst2[:, :])
        S.op('dve', 'tensor_copy', ['cf'], ['cb'], out=cb[:], in_=cf[:, 0:512])
        identf = cf[:, 0:128]
        onesf = cf[:, 384:512]
        iota_row = cf[:, 512:768]
        identb = cb[:, 0:128]
        trib = cb[:, 128:256]
        utrib = cb[:, 256:384]
        onesb = cb[:, 384:512]
        iota_p2 = c2[:, 0:2]
        kbias = c2[:, 2:18]
        down = c2[:, 18:26]

        accraw = sb(st, "accraw", [128, 2 * NT * DM], BF16)
        acc = accraw[:].bitcast(F32).rearrange("p (a d) -> p a d", d=DM)
        S.barrier()

        try:
            with ExitStack() as s1:
                convT = sb(s1, "convT", [128, 8, TOK], BF16)
                kmean = sb(s1, "kmean", [128, NH, 32])
                w_in_v = w_in.rearrange("(c p) f -> p c f", p=128)
                accf = accraw[:].bitcast(F32)
                accb = accraw[:]

                with ExitStack() as sc:
                    wga = [sb(sc, "wga%d" % i, [128, 16, 128], BF16) for i in range(2)]
                    wgb = [sb(sc, "wgb%d" % i, [128, 16, 128], BF16) for i in range(2)]
                    cw = sb(sc, "cw", [128, 8, 31])
                    cv = sb(sc, "cv", [128, 24])
                    sg = [sb(sc, "sg%d" % i, [128, 1056]) for i in range(2)]
                    ug = [sb(sc, "ug%d" % i, [128, 1056]) for i in range(2)]
                    sq = [sb(sc, "sq%d" % i, [128, 1024]) for i in range(2)]
                    xto = sb(sc, "xto_c", [128, 16, 1056], BF16)
                    S.dma('pool', [], ['xto'], out=xto[:], in_=xTo.rearrange("(c p) t -> p c t", p=128))
                    cT = accf[:, 0:8192].rearrange("p (g t) -> p g t", t=TOK)
                    ty = [accf[:, 8192 + i * 1024:8192 + (i + 1) * 1024] for i in range(2)]
                    tz = [accf[:, 10240 + i * 1024:10240 + (i + 1) * 1024] for i in range(2)]
                    mean = accf[:, 12288:13312]
                    rstd = accf[:, 13312:14336]
                    msq = accf[:, 14336:15360]
                    S.dma('sp', [], ['cw'], out=cw[:], in_=convw.rearrange("p (g j) -> p g j", j=31))
                    S.dma('sp', [], ['cv'], out=cv[:], in_=convv[:, :])
                    for g in range(8):
                        b = g % 2
                        S.dma('pool', [], [('wga', b)], out=wga[b][:], in_=w_in_v[:, :, 3072 + g * 128:3072 + (g + 1) * 128])
                        S.dma('pool', [], [('wgb', b)], out=wgb[b][:], in_=w_in_v[:, :, 4096 + g * 128:4096 + (g + 1) * 128])
                        for k in range(3):
                            pa = k % 2
                            pbb = 2 + k % 2
                            cs = slice(k * 352, (k + 1) * 352)
                            for c in range(16):
                                S.op('pe', 'matmul', [('wga', b), 'xto'], [('ps', pa)], PS[pa][:, 0:352],
                                     lhsT=wga[b][:, c, :], rhs=xto[:, c, cs], start=(c == 0), stop=(c == 15))
                            for c in range(16):
                                S.op('pe', 'matmul', [('wgb', b), 'xto'], [('ps', pbb)], PS[pbb][:, 0:352],
                                     lhsT=wgb[b][:, c, :], rhs=xto[:, c, cs], start=(c == 0), stop=(c == 15))
                            S.op('act', 'activation', [('ps', pbb)], [('sg', b, k)], out=sg[b][:, cs], in_=PS[pbb][:, 0:352], func=AF.Sigmoid)
                            S.op('dve', 'tensor_tensor', [('ps', pa), ('sg', b, k)], [('ug', b, k)], out=ug[b][:, cs], in0=PS[pa][:, 0:352], in1=sg[b][:, cs], op=ALU.mult)
                        ukeys = [('ug', b, k) for k in range(3)]
                        S.op('dve', 'tensor_scalar', ukeys + ['cw', 'cv'], [('cT', g)], out=cT[:, g, :], in0=ug[b][:, 2:2 + TOK],
                             scalar1=cw[:, g, 0:1], scalar2=cv[:, g:g + 1], op0=ALU.mult, op1=ALU.add)
                        for j in range(1, 31):
                            S.op('dve', 'scalar_tensor_tensor', ukeys + ['cw', ('cT', g)], [('cT', g)], out=cT[:, g, :], in0=ug[b][:, j + 2:j + 2 + TOK],
                                 scalar=cw[:, g, j:j + 1], in1=cT[:, g, :], op0=ALU.mult, op1=ALU.add)
                        S.op('act', 'activation', [('cT', g)], [('sq', b)], out=sq[b][:], in_=cT[:, g, :], func=AF.Square)
                        for half in range(2):
                            hs = slice(half * 512, (half + 1) * 512)
                            S.op('pe', 'matmul', [('cT', g)], [('ps', 4 + half)], PS[4 + half][:], lhsT=onesf, rhs=cT[:, g, hs], start=(g == 0), stop=(g == 7))
                            S.op('pe', 'matmul', [('sq', b)], [('ps', 6 + half)], PS[6 + half][:], lhsT=onesf, rhs=sq[b][:, hs], start=(g == 0), stop=(g == 7))
                    for half in range(2):
                        hs = slice(half * 512, (half + 1) * 512)
                        S.op('act', 'activation', [('ps', 4 + half)], [('mean', half)], out=mean[:, hs], in_=PS[4 + half][:], func=AF.Copy, scale=1.0 / 1024.0)
                        S.op('dve', 'tensor_tensor', [('mean', half)], [('msq', half)], out=msq[:, hs], in0=mean[:, hs], in1=mean[:, hs], op=ALU.mult)
                        S.op('dve', 'scalar_tensor_tensor', [('ps', 6 + half), ('msq', half)], [('rstd', half)], out=rstd[:, hs], in0=PS[6 + half][:],
                             scalar=1.0 / 1024.0, in1=msq[:, hs], op0=ALU.mult, op1=ALU.subtract)
                        S.op('dve', 'tensor_scalar', [('rstd', half)], [('rstd', half)], out=rstd[:, hs], in0=rstd[:, hs], scalar1=EPS, scalar2=None, op0=ALU.add)
                        S.op('act', 'activation', [('rstd', half)], [('rstd', half)], out=rstd[:, hs], in_=rstd[:, hs], func=AF.Sqrt)
                        S.op('dve', 'reciprocal', [('rstd', half)], [('rstd', half)], out=rstd[:, hs], in_=rstd[:, hs])
                    for g in range(8):
                        b = g % 2
                        S.op('dve', 'tensor_tensor', [('cT', g), ('mean', 0), ('mean', 1)], [('ty', b)], out=ty[b][:], in0=cT[:, g, :], in1=mean[:], op=ALU.subtract)
                        S.op('dve', 'tensor_tensor', [('ty', b), ('rstd', 0), ('rstd', 1)], [('ty', b)], out=ty[b][:], in0=ty[b][:], in1=rstd[:], op=ALU.mult)
                        S.op('act', 'activation', [('ty', b), 'cv'], [('tz', b)], out=tz[b][:], in_=ty[b][:], func=AF.Identity, scale=cv[:, 8 + g:9 + g], bias=cv[:, 16 + g:17 + g])
                        S.op('act', 'activation', [('tz', b)], [('ty', b)], out=ty[b][:], in_=tz[b][:], func=AF.Sigmoid)
                        S.op('dve', 'tensor_tensor', [('ty', b), ('tz', b)], [('convT', g)], out=convT[:, g, :], in0=tz[b][:], in1=ty[b][:], op=ALU.mult)
                    S.barrier()
                    if stop == 1:
                        S.dma('sp', [], ['dbgb1'], out=dbgb[:, 0:8192], in_=convT[:].rearrange('p a d -> p (a d)'))
                        raise _Stop()

                with ExitStack() as sa:
                    Wk = accb[:, 0:16384].rearrange("p (c f) -> p c f", f=1024)
                    Wv = accb[:, 16384:32768].rearrange("p (c f) -> p c f", f=1024)
                    for cg in range(4):
                        S.dma('pool', [], [('Wk', cg)], out=Wk[:, cg * 4:(cg + 1) * 4, :], in_=w_in_v[:, cg * 4:(cg + 1) * 4, 1024:2048])
                        S.dma('pool', [], [('Wv', cg)], out=Wv[:, cg * 4:(cg + 1) * 4, :], in_=w_in_v[:, cg * 4:(cg + 1) * 4, 2048:3072])
                    with ExitStack() as sa1:
                        xc = [sb(sa1, "xc%d" % i, [128, 16, 512], BF16) for i in range(2)]
                        ktsb = [sb(sa1, "ktsb%d" % i, [128, 512], BF16) for i in range(3)]
                        vsb = [sb(sa1, "vsb%d" % i, [128, 1024], BF16) for i in range(2)]
                        kmsum = sb(sa1, "kmsum", [128, NH, 32])
                        xT_v = xT.rearrange("(c p) t -> p c t", p=128)
                        ke = 0
                        ve = 0
                        for tcn in range(16):
                            b = tcn % 2
                            S.dma('pool', [], [('xc', b)], out=xc[b][:], in_=xT_v[:, :, tcn * 512:(tcn + 1) * 512])
                            for h in range(NH if not os.environ.get('A_NOK') else 0):
                                pb = h % 2
                                for c in range(16):
                                    S.op('pe', 'matmul', [('Wk', c // 4), ('xc', b)], [('ps', pb)], PS[pb][:],
                                         lhsT=Wk[:, c, h * 128:(h + 1) * 128], rhs=xc[b][:, c, :],
                                         start=(c == 0), stop=(c == 15))
                                kb = ke % 3
                                ke += 1
                                S.op('act', 'activation', [('ps', pb)], [('ktsb', kb)], out=ktsb[kb][:], in_=PS[pb][:], func=AF.Copy)
                                if not os.environ.get('A_NORED'):
                                  S.op('dve', 'tensor_reduce', [('ktsb', kb)], [('kmsum', h, tcn)],
                                     out=kmsum[:, h, 2 * tcn:2 * tcn + 2],
                                     in_=ktsb[kb][:].rearrange("p (b t) -> p b t", t=256), axis=AX.X, op=ALU.add)
                                if not os.environ.get('SKIP_SCR'):
                                  S.dma('sp', [('ktsb', kb)], [('KTs', h, tcn)], out=KTs[h, :, tcn * 512:(tcn + 1) * 512], in_=ktsb[kb][:])
                            for s in range(4 if not os.environ.get('A_NOV') else 0):
                                vb = ve % 2
                                ve += 1
                                for half in range(2):
                                    pb = 2 + half
                                    for c in range(16):
                                        S.op('pe', 'matmul', [('Wv', c // 4), ('xc', b)], [('ps', pb)], PS[pb][:],
                                             lhsT=xc[b][:, c, s * 128:(s + 1) * 128], rhs=Wv[:, c, half * 512:(half + 1) * 512],
                                             start=(c == 0), stop=(c == 15))
                                    if half == 0:
                                        S.op('act', 'activation', [('ps', pb)], [('vsb', vb, half)], out=vsb[vb][:, 0:512], in_=PS[pb][:], func=AF.Copy)
                                    else:
                                        S.op('dve', 'tensor_copy', [('ps', pb)], [('vsb', vb, half)], out=vsb[vb][:, 512:1024], in_=PS[pb][:])
                                t0 = tcn * 512 + s * 128
                                if not os.environ.get('SKIP_SCR'):
                                  S.dma('sp', [('vsb', vb, 0), ('vsb', vb, 1)], [('Vs', t0)], out=Vs[t0:t0 + 128, :], in_=vsb[vb][:])
                        S.op('dve', 'tensor_scalar', [('kmsum', h, t) for h in range(NH) for t in range(16)], ['kmean'],
                             out=kmean[:], in0=kmsum[:], scalar1=1.0 / 256.0, scalar2=None, op0=ALU.mult)
                        S.barrier()
                        if stop == 2:
                            dump_f(kmean[:].rearrange('p a b -> p (a b)'), 2048, 256)
                            raise _Stop()

                    QT = sb(s1, "QT", [128, NH, TOK], BF16)
                    KTo = sb(s1, "KTo", [128, NH, TOK], BF16)
                    Vo = sb(s1, "Vo", [128, NT, NH, 129], BF16)
                    Wt = sb(s1, "Wt", [128, NH, NT, 32])
                    with ExitStack() as sa2:
                        xto = sb(sa2, "xto_a", [128, 16, 1056], BF16)
                        S.dma('pool', [], ['xto'], out=xto[:], in_=xTo.rearrange("(c p) t -> p c t", p=128))
                        Wqh = [sb(sa2, "Wq%d" % i, [128, 16, 128], BF16) for i in range(2)]
                        qf = [sb(sa2, "qf%d" % i, [128, 512]) for i in range(2)]
                        dt_sb = sb(sa2, "dt_sb", [128, NT, 32])
                        fp_sb = sb(sa2, "fp_sb", [128, NT, 32])
                        gm = [sb(sa2, "gm%d" % i, [128, 32]) for i in range(2)]
                        t8 = [sb(sa2, "t8%d" % i, [128, 8]) for i in range(2)]
                        thr = [sb(sa2, "thr%d" % i, [128, 1]) for i in range(2)]
                        sel = [sb(sa2, "sel%d" % i, [128, 32]) for i in range(2)]
                        ex = [sb(sa2, "ex%d" % i, [128, 32]) for i in range(2)]
                        S.dma('sp', [], ['dt_sb'], out=dt_sb[:], in_=dtab.rearrange("p (i n) -> p i n", n=32))
                        S.dma('sp', [], ['fp_sb'], out=fp_sb[:], in_=fptab.rearrange("p (i n) -> p i n", n=32))
                        S.op('pool', 'memset', [], [('Vo1',)], Vo[:, :, :, 128:129], 1.0)
                        cnt = 0
                        for h in range(NH):
                            S.dma('pool', [], [('Wq', h % 2)], out=Wqh[h % 2][:], in_=w_in_v[:, :, h * 128:(h + 1) * 128])
                            for qc in range(2):
                                pb = qc
                                for c in range(16):
                                    S.op('pe', 'matmul', [('Wq', h % 2), 'xto'], [('ps', pb)], PS[pb][:],
                                         lhsT=Wqh[h % 2][:, c, :], rhs=xto[:, c, 32 + qc * 512:32 + (qc + 1) * 512],
                                         start=(c == 0), stop=(c == 15))
                                S.op('dve', 'tensor_copy', [('ps', pb)], [('qf', qc)], out=qf[qc][:], in_=PS[pb][:])
                                S.op('act', 'activation', [('qf', qc)], [('QT', h, qc)], out=QT[:, h, qc * 512:(qc + 1) * 512], in_=qf[qc][:], func=AF.Copy)
                                for j in range(4):
                                    i = qc * 4 + j
                                    k2 = cnt % 2
                                    cnt += 1
                                    gp = PS[4 + k2][:, 0:32]
                                    S.op('pe', 'matmul', [('qf', qc), 'kmean'], [('ps', 4 + k2)], gp,
                                         lhsT=qf[qc][:, j * 128:(j + 1) * 128], rhs=kmean[:, h, :], start=True, stop=True)
                                    S.op('dve', 'tensor_tensor', [('ps', 4 + k2), 'fp_sb'], [('gm', k2)], out=gm[k2][:], in0=gp, in1=fp_sb[:, i, :], op=ALU.add)
                                    S.op('dve', 'max', [('gm', k2)], [('t8', k2)], out=t8[k2][:], in_=gm[k2][:])
                                    S.op('dve', 'tensor_scalar', [('t8', k2)], [('thr', k2)], out=thr[k2][:], in0=t8[k2][:, 2:3], scalar1=-1.0e29, scalar2=None, op0=ALU.max)
                                    S.op('dve', 'tensor_scalar', [('gm', k2), ('thr', k2)], [('sel', k2)], out=sel[k2][:], in0=gm[k2][:], scalar1=thr[k2][:, 0:1], scalar2=None, op0=ALU.is_ge)
                                    S.op('act', 'activation', ['dt_sb'], [('ex', k2)], out=ex[k2][:], in_=dt_sb[:, i, :], func=AF.Exp, scale=SLOPES[h])
                                    S.op('dve', 'tensor_tensor', [('sel', k2), ('ex', k2)], [('Wt', h, i)], out=Wt[:, h, i, :], in0=sel[k2][:], in1=ex[k2][:], op=ALU.mult)
                            for qc in range(2):
                                pb = 2 + qc
                                for c in range(16):
                                    S.op('pe', 'matmul', ['Wk', 'xto'], [('ps', pb)], PS[pb][:],
                                         lhsT=Wk[:, c, h * 128:(h + 1) * 128], rhs=xto[:, c, 32 + qc * 512:32 + (qc + 1) * 512],
                                         start=(c == 0), stop=(c == 15))
                                S.op('act', 'activation', [('ps', pb)], [('KTo', h, qc)], out=KTo[:, h, qc * 512:(qc + 1) * 512], in_=PS[pb][:], func=AF.Copy)
                        for i in range(NT):
                            for half in range(2):
                                pb = 6 + half
                                for c in range(16):
                                    S.op('pe', 'matmul', ['Wv', 'xto'], [('ps', pb)], PS[pb][:],
                                         lhsT=xto[:, c, 32 + i * 128:32 + (i + 1) * 128], rhs=Wv[:, c, half * 512:(half + 1) * 512],
                                         start=(c == 0), stop=(c == 15))
                                S.op('dve' if half else 'act', 'tensor_copy' if half else 'activation', [('ps', pb)], [('Vo', i, half)],
                                     **(dict(out=Vo[:, i, half * 4:(half + 1) * 4, 0:128], in_=PS[pb][:].rearrange("p (h d) -> p h d", d=128))
                                        if half else dict(out=Vo[:, i, 0:4, 0:128], in_=PS[pb][:].rearrange("p (h d) -> p h d", d=128), func=AF.Copy)))
                        S.barrier()
                        if stop == 3:
                            S.dma('sp', [], ['dbgb3'], out=dbgb[:, 0:8192], in_=QT[:].rearrange('p a d -> p (a d)'))
                            S.dma('sp', [], ['dbgb3b'], out=dbgb[:, 8192:16384], in_=KTo[:].rearrange('p a d -> p (a d)'))
                            dump_f(Wt[:].rearrange('p a b c -> p (a b c)'), 0, 2048)
                            dump_f(kmean[:].rearrange('p a b -> p (a b)'), 2048, 256)
                            raise _Stop()

                attnT = sb(s1, "attnT", [128, NH, TOK], BF16)
                with ExitStack() as sbk:
                    KTh = [accb[:, i * 8192:(i + 1) * 8192] for i in range(2)]
                    Vh = [accb[:, 16384:16384 + 64 * 129].rearrange("p (n d) -> p n d", d=129), sb(sbk, "Vh1", [128, 64, 129], BF16)]
                    PT = [sb(sbk, "PT%d" % i, [128, 512], BF16) for i in range(4)]
                    PO = [sb(sbk, "PO%d" % i, [128, 128], BF16) for i in range(2)]
                    ac = sb(sbk, "ac", [128, NT, 129])
                    wown = sb(sbk, "wown", [128, NT])
                    rec = sb(sbk, "rec", [128, NT])
                    atok = [sb(sbk, "atok%d" % i, [128, 128], BF16) for i in range(2)]
                    Vs_v = Vs.rearrange("(n p) d -> p n d", p=128)
                    for i in range(2):
                        S.op('pool', 'memset', [], [('Vh1', i)], Vh[i][:, :, 128:129], 1.0)
                    pti = 0
                    oi = 0
                    for h in range(NH):
                        hb = h % 2
                        S.dma('sp', [], [('KTh', hb)], out=KTh[hb][:], in_=KTs[h, :, :])
                        for vg in range(8):
                            S.dma('sp', [('Vh1', hb)], [('Vh', hb, vg)], out=Vh[hb][:, vg * 8:(vg + 1) * 8, 0:128], in_=Vs_v[:, vg * 8:(vg + 1) * 8, h * 128:(h + 1) * 128])
                        S.op('dve', 'memset', [], [('ac', i) for i in range(NT)], ac[:], 0.0)
                        S.op('act', 'activation', ['c2'], ['wown'], out=wown[:], in_=down, func=AF.Exp, scale=SLOPES[h])
                        steps = [(n, qc) for n in range(31) for qc in range(2)]

                        def emit_st(n, qc):
                            nonlocal pti
                            pts = []
                            for kk in range(2):
                                pb = pti % 4
                                pti += 1
                                S.op('pe', 'matmul', [('KTh', hb), ('QT', h, qc)], [('ps', pb)], PS[pb][:],
                                     lhsT=KTh[hb][:, (2 * n + kk) * 128:(2 * n + kk + 1) * 128], rhs=QT[:, h, qc * 512:(qc + 1) * 512], start=True, stop=True)
                                pts.append(pb)
                            return pts
                        nxt = emit_st(*steps[0])
                        for si, (n, qc) in enumerate(steps):
                            pts = nxt
                            for kk in range(2):
                                pb = pts[kk]
                                S.op('act', 'activation', [('ps', pb), 'c2'], [('PT', pb)], out=PT[pb][:], in_=PS[pb][:], func=AF.Exp,
                                     scale=SCALE, bias=kbias[:, 2 * h + kk:2 * h + kk + 1])
                            if si + 1 < len(steps):
                                nxt = emit_st(*steps[si + 1])
                            for qs in range(4):
                                i = qc * 4 + qs
                                ob = oi % 4
                                oi += 1
                                ops_ = PS[4 + ob][:, 0:129]
                                for kk in range(2):
                                    S.op('pe', 'matmul', [('PT', pts[kk]), ('Vh', hb, (2 * n + kk) // 8)], [('po', ob)], ops_,
                                         lhsT=PT[pts[kk]][:, qs * 128:(qs + 1) * 128], rhs=Vh[hb][:, 2 * n + kk, :], start=(kk == 0), stop=(kk == 1))
                                S.op('dve', 'scalar_tensor_tensor', [('po', ob), ('Wt', h, i), ('ac', i)], [('ac', i)], out=ac[:, i, :], in0=ops_,
                                     scalar=Wt[:, h, i, n:n + 1], in1=ac[:, i, :], op0=ALU.mult, op1=ALU.add)
                        for i in range(NT):
                            ob = oi % 4
                            oi += 1
                            ops_ = PS[4 + ob][:, 0:129]
                            nk = i % 2 + 1
                            for kk in range(nk):
                                pb = pti % 4
                                pti += 1
                                k0 = (i // 2) * 256 + kk * 128
                                S.op('pe', 'matmul', [('KTo', h, k0 // 512), ('QT', h, i // 4)], [('ps', pb)], PS[pb][:, 0:128],
                                     lhsT=KTo[:, h, k0:k0 + 128], rhs=QT[:, h, i * 128:(i + 1) * 128], start=True, stop=True)
                                po = (pti) % 2
                                S.op('act', 'activation', [('ps', pb), 'c2'], [('PO', po)], out=PO[po][:], in_=PS[pb][:, 0:128], func=AF.Exp,
                                     scale=SCALE, bias=kbias[:, 2 * h + kk:2 * h + kk + 1])
                                if kk == i % 2:
                                    S.op('dve', 'tensor_tensor', [('PO', po), 'cb'], [('PO', po)], out=PO[po][:], in0=PO[po][:], in1=trib, op=ALU.mult)
                                vt = (i // 2) * 2 + kk
                                S.op('pe', 'matmul', [('PO', po), ('Vo', vt, h // 4), ('Vo1',)], [('po', ob)], ops_,
                                     lhsT=PO[po][:], rhs=Vo[:, vt, h, :], start=(kk == 0), stop=(kk == nk - 1))
                            S.op('dve', 'scalar_tensor_tensor', [('po', ob), 'wown', ('ac', i)], [('ac', i)], out=ac[:, i, :], in0=ops_,
                                 scalar=wown[:, i:i + 1], in1=ac[:, i, :], op0=ALU.mult, op1=ALU.add)
                        S.op('dve', 'reciprocal', [('ac', i) for i in range(NT)], ['rec'], out=rec[:], in_=ac[:, :, 128])
                        for i in range(NT):
                            ab = i % 2
                            S.op('dve', 'tensor_scalar', [('ac', i), 'rec'], [('atok', ab)], out=atok[ab][:], in0=ac[:, i, 0:128], scalar1=rec[:, i:i + 1], scalar2=None, op0=ALU.mult)
                            pb = pti % 4
                            pti += 1
                            S.op('pe', 'matmul', [('atok', ab), 'cb'], [('ps', pb)], PS[pb][:, 0:128], lhsT=atok[ab][:], rhs=identb, start=True, stop=True)
                            S.op('act', 'activation', [('ps', pb)], [('attnT', h, i)], out=attnT[:, h, i * 128:(i + 1) * 128], in_=PS[pb][:, 0:128], func=AF.Copy)
                    S.barrier()
                    if stop == 4:
                        S.dma('sp', [], ['dbgb4'], out=dbgb[:, 0:8192], in_=attnT[:].rearrange('p a d -> p (a d)'))
                        raise _Stop()

                with ExitStack() as sc3:
                    wo = [sb(sc3, "wo%d" % i, [128, 16, 512], BF16) for i in range(2)]
                    xr = [sb(sc3, "xr%d" % i, [128, 512]) for i in range(3)]
                    w_out_v = w_out.rearrange("(c p) f -> p c f", p=128)
                    xi = 0
                    for cc in range(4):
                        b = cc % 2
                        S.dma('pool', [], [('wo', b)], out=wo[b][:], in_=w_out_v[:, :, cc * 512:(cc + 1) * 512])
                        for i in range(NT):
                            pb = i % 4
                            xb = xi % 3
                            xi += 1
                            S.dma('sp', [], [('xr', xb)], out=xr[xb][:], in_=xo[i * 128:(i + 1) * 128, cc * 512:(cc + 1) * 512])
                            for c in range(16):
                                src = attnT if c < 8 else convT
                                S.op('pe', 'matmul', [('wo', b)], [('ps', pb)], PS[pb][:],
                                     lhsT=src[:, c % 8, i * 128:(i + 1) * 128], rhs=wo[b][:, c, :], start=(c == 0), stop=(c == 15))
                            S.op('dve', 'scalar_tensor_tensor', [('ps', pb), ('xr', xb)], [('acc', i, cc)], out=acc[:, i, cc * 512:(cc + 1) * 512], in0=xr[xb][:],
                                 scalar=ALPHA, in1=PS[pb][:], op0=ALU.mult, op1=ALU.add)
                    S.barrier()
                    if stop == 5:
                        dump_f(accraw[:].bitcast(F32), 0, 16384)
                        raise _Stop()

            def layer_norm_tile(src_ap, dst_ap, gb, stats, mv, rs, tmp, keys_r, keys_w, tag):
                for k in range(4):
                    S.op('dve', 'bn_stats', keys_r, [(tag, 'st', k)], out=stats[:, k, :], in_=src_ap[:, k * 512:(k + 1) * 512])
                S.op('dve', 'bn_aggr', [(tag, 'st', k) for k in range(4)], [(tag, 'mv')], out=mv[:], in_=stats[:].rearrange("p a b -> p (a b)"))
                S.op('dve', 'tensor_scalar', [(tag, 'mv')], [(tag, 'rs')], out=rs[:], in0=mv[:, 1:2], scalar1=EPS, scalar2=None, op0=ALU.add)
                S.op('act', 'activation', [(tag, 'rs')], [(tag, 'rs')], out=rs[:], in_=rs[:], func=AF.Sqrt)
                S.op('dve', 'reciprocal', [(tag, 'rs')], [(tag, 'rs')], out=rs[:], in_=rs[:])
                S.op('dve', 'tensor_scalar', keys_r + [(tag, 'mv'), (tag, 'rs')], [(tag, 'tmp')], out=tmp[:], in0=src_ap, scalar1=mv[:, 0:1], scalar2=rs[:, 0:1],
                     op0=ALU.subtract, op1=ALU.mult)
                S.op('dve', 'tensor_tensor', [(tag, 'tmp'), tag + 'gb'], [(tag, 'tmp')], out=tmp[:], in0=tmp[:], in1=gb[:, 0, :], op=ALU.mult)
                S.op('dve', 'tensor_tensor', [(tag, 'tmp'), tag + 'gb'], keys_w, out=dst_ap, in0=tmp[:], in1=gb[:, 1, :], op=ALU.add)

            with ExitStack() as s2:
                h1b = sb(s2, "h1b", [128, NT, DM], BF16)
                gate = sb(s2, "gate", [128, NT, NE])
                rankm = sb(s2, "rankm", [128, NT, NE])
                gateT = sb(s2, "gateT", [NE, TOK])
                bgs = sb(s2, "bgs", [128, NE * 32])
                S.dma('sp', [], ['bgs'], out=bgs[:], in_=bgu[:, :])
                with ExitStack() as sr:
                    gb1 = sb(sr, "gb1", [128, 2, DM])
                    stats = sb(sr, "stats", [128, 4, 6])
                    mv = sb(sr, "mv", [128, 2])
                    rs = sb(sr, "rs", [128, 1])
                    tmp = sb(sr, "tmp", [128, DM])
                    h1f = [sb(sr, "h1f%d" % i, [128, DM]) for i in range(2)]
                    hT = [sb(sr, "hT%d" % i, [128, 128]) for i in range(4)]
                    wr = sb(sr, "wr", [128, 16, NE])
                    brb = sb(sr, "brb", [128, NE])
                    lg = sb(sr, "lg", [128, NE])
                    t8 = sb(sr, "t8r", [128, 8])
                    nmx = sb(sr, "nmx", [128, 1])
                    selt = sb(sr, "selt", [128, NT, NE])
                    selb = sb(sr, "selb", [128, NT, NE], BF16)
                    exr = sb(sr, "exr", [128, NE])
                    den = sb(sr, "den", [128, 1])
                    rkT = sb(sr, "rkT", [NE, TOK])
                    slT = sb(sr, "slT", [NE, TOK])
                    bds = sb(sr, "bds", [NE, DM])
                    tr = sb(sr, "tr", [128, NE])
                    S.dma('sp', [], ['ln1gb'], out=gb1[:].rearrange("p a d -> p (a d)"), in_=ln1.rearrange("a d -> (a d)").partition_broadcast(128))
                    S.dma('sp', [], ['wr'], out=wr[:], in_=w_r.rearrange("(c p) e -> p c e", p=128))
                    S.dma('sp', [], ['brb'], out=brb[:], in_=b_r[0, :].partition_broadcast(128))
                    S.dma('sp', [], ['bds'], out=bds[:], in_=b_dn[:, :])
                    hti = 0
                    for i in range(NT):
                        fb = i % 2
                        layer_norm_tile(acc[:, i, :], h1f[fb][:], gb1, stats, mv, rs, tmp, [('acc', i)], [('h1f', fb)], 'ln1')
                        S.op('act', 'activation', [('h1f', fb)], [('h1b', i)], out=h1b[:, i, :], in_=h1f[fb][:], func=AF.Copy)
                        S.op('act', 'activation', [('h1f', fb)], [('acc', i)], out=acc[:, i, :], in_=h1f[fb][:], func=AF.Copy, scale=ALPHA)
                        for c in range(16):
                            pb = c % 4
                            S.op('pe', 'matmul', [('h1f', fb), 'cf'], [('ps', pb)], PS[pb][:, 0:128], lhsT=h1f[fb][:, c * 128:(c + 1) * 128], rhs=identf, start=True, stop=True)
                            tb = hti % 4
                            hti += 1
                            S.op('act' if c % 2 else 'dve', 'activation' if c % 2 else 'tensor_copy', [('ps', pb)], [('hT', tb)],
                                 **(dict(out=hT[tb][:], in_=PS[pb][:, 0:128], func=AF.Copy) if c % 2 else dict(out=hT[tb][:], in_=PS[pb][:, 0:128])))
                            S.op('pe', 'matmul', [('hT', tb), 'wr'], [('ps', 4)], PS[4][:, 0:NE], lhsT=hT[tb][:], rhs=wr[:, c, :], start=(c == 0), stop=(c == 15))
                        S.op('dve', 'tensor_tensor', [('ps', 4), 'brb'], ['lg'], out=lg[:], in0=PS[4][:, 0:NE], in1=brb[:], op=ALU.add)
                        S.op('dve', 'max', ['lg'], ['t8'], out=t8[:], in_=lg[:])
                        S.op('dve', 'tensor_scalar', ['lg', 't8'], [('selt', i)], out=selt[:, i, :], in0=lg[:], scalar1=t8[:, 3:4], scalar2=None, op0=ALU.is_ge)
                        S.op('dve', 'tensor_scalar', ['t8'], ['nmx'], out=nmx[:], in0=t8[:, 0:1], scalar1=-1.0, scalar2=None, op0=ALU.mult)
                        S.op('act', 'activation', ['lg', 'nmx'], ['exr'], out=exr[:], in_=lg[:], func=AF.Exp, bias=nmx[:, 0:1], scale=1.0)
                        S.op('dve', 'tensor_tensor', ['exr', ('selt', i)], ['exr'], out=exr[:], in0=exr[:], in1=selt[:, i, :], op=ALU.mult)
                        S.op('dve', 'tensor_reduce', ['exr'], ['den'], out=den[:], in_=exr[:], axis=AX.X, op=ALU.add)
                        S.op('dve', 'reciprocal', ['den'], ['den'], out=den[:], in_=den[:])
                        S.op('dve', 'tensor_scalar', ['exr', 'den'], [('gate', i)], out=gate[:, i, :], in0=exr[:], scalar1=den[:, 0:1], scalar2=None, op0=ALU.mult)
                        S.op('dve', 'tensor_copy', [('selt', i)], [('selb', i)], out=selb[:, i, :], in_=selt[:, i, :])
                    for i in range(NT):
                        for i2 in range(i + 1):
                            S.op('pe', 'matmul', [('selb', i2), 'cb'], [('ps', 5)], PS[5][:, 0:NE], lhsT=(utrib if i2 == i else onesb), rhs=selb[:, i2, :], start=(i2 == 0), stop=(i2 == i))
                        S.op('dve', 'tensor_tensor', [('ps', 5), ('selt', i)], ['tr'], out=tr[:], in0=PS[5][:, 0:NE], in1=selt[:, i, :], op=ALU.mult)
                        S.op('dve', 'scalar_tensor_tensor', ['tr', ('selt', i)], [('rankm', i)], out=rankm[:, i, :], in0=selt[:, i, :], scalar=-1.0, in1=tr[:], op0=ALU.add, op1=ALU.add)
                        for i2 in range(i + 1):
                            S.op('pe', 'matmul', [('selb', i2), 'cb'], [('ps', 6)], PS[6][0:NE, 0:128], lhsT=selb[:, i2, :], rhs=(utrib if i2 == i else onesb), start=(i2 == 0), stop=(i2 == i))
                        S.op('pe', 'matmul', [('selb', i), 'cb'], [('ps', 7)], PS[7][0:NE, 0:128], lhsT=selb[:, i, :], rhs=identb, start=True, stop=True)
                        S.op('pe', 'matmul', [('gate', i), 'cf'], [('ps', 3)], PS[3][0:NE, 0:128], lhsT=gate[:, i, :], rhs=identf, start=True, stop=True)
                        ts_ = slice(i * 128, (i + 1) * 128)
                        S.op('act', 'activation', [('ps', 7)], [('slT', i)], out=slT[:, ts_], in_=PS[7][0:NE, 0:128], func=AF.Copy)
                        S.op('act', 'activation', [('ps', 3)], [('gateT', i)], out=gateT[:, ts_], in_=PS[3][0:NE, 0:128], func=AF.Copy)
                        S.op('dve', 'tensor_tensor', [('ps', 6), ('slT', i)], [('rkT', i)], out=rkT[:, ts_], in0=PS[6][0:NE, 0:128], in1=slT[:, ts_], op=ALU.mult)
                        S.op('dve', 'scalar_tensor_tensor', [('rkT', i), ('slT', i)], [('rkT', i)], out=rkT[:, ts_], in0=slT[:, ts_], scalar=-1.0, in1=rkT[:, ts_], op0=ALU.add, op1=ALU.add)
                    S.dma('sp', [('rkT', i) for i in range(NT)], ['RK'], out=RK[:, :], in_=rkT[:])
                    S.dma('sp', [('gateT', i) for i in range(NT)], ['GT'], out=GT[:, :], in_=gateT[:])
                    k = 0
                    for i in range(NT):
                        for cc in range(4):
                            pb = k % 4
                            k += 1
                            S.op('pe', 'matmul', [('gateT', i), 'bds'], [('ps', pb)], PS[pb][:], lhsT=gateT[:, i * 128:(i + 1) * 128], rhs=bds[:, cc * 512:(cc + 1) * 512], start=True, stop=True)
                            S.op('dve', 'tensor_tensor', [('ps', pb), ('acc', i)], [('acc', i)], out=acc[:, i, cc * 512:(cc + 1) * 512], in0=PS[pb][:], in1=acc[:, i, cc * 512:(cc + 1) * 512], op=ALU.add)
                    S.barrier()
                    if stop == 6:
                        dump_f(accraw[:].bitcast(F32), 0, 16384)
                        S.dma('sp', [], ['dbgb6'], out=dbgb[:, 0:16384], in_=h1b[:].rearrange('p a d -> p (a d)'))
                        raise _Stop()

                with ExitStack() as sd:
                    RW = 7
                    wb = [sb(sd, "wb%d" % i, [128, 16, 256], BF16) for i in range(RW)]
                    wi = 0
                    xg = sb(sd, "xg", [128, 16, CAP], BF16)
                    actT = sb(sd, "actT", [128, 16, CAP], BF16)
                    selE = [sb(sd, "selE%d" % i, [128, NT, CAP], BF16) for i in range(1)]
                    selG = [sb(sd, "selG%d" % i, [128, 2, TOK], BF16) for i in range(1)]
                    rbc = [sb(sd, "rbc%d" % i, [128, TOK]) for i in range(1)]
                    gbc = [sb(sd, "gbc%d" % i, [128, TOK]) for i in range(1)]
                    yb = [sb(sd, "yb%d" % i, [128, 2, 256], BF16) for i in range(2)]
                    g1 = [sb(sd, "g1%d" % i, [128, CAP]) for i in range(2)]
                    sgm = [sb(sd, "sgm%d" % i, [128, CAP]) for i in range(2)]
                    u1 = [sb(sd, "u1%d" % i, [128, CAP]) for i in range(2)]
                    gui = 0
                    dni = 0
                    xgi = 0
                    pri = 0
                    pend = None
                    for e in range(NE):
                        eb = 0
                        S.dma('sp', ['RK'], [('rbc', eb)], out=rbc[eb][:], in_=RK[e, :].partition_broadcast(128))
                        S.dma('sp', ['GT'], [('gbc', eb)], out=gbc[eb][:], in_=GT[e, :].partition_broadcast(128))
                        for i in range(NT):
                            S.op('dve', 'tensor_scalar', ['cf', ('rankm', i)], [('selE', eb, i)], out=selE[eb][:, i, :], in0=iota_row[:, 0:CAP], scalar1=rankm[:, i, e:e + 1], scalar2=None, op0=ALU.is_equal)
                        for j in range(2):
                            mj = MJ[j]
                            S.op('dve', 'scalar_tensor_tensor', [('rbc', eb), ('gbc', eb), 'c2'], [('selG', eb, j)], out=selG[eb][0:mj, j, :], in0=rbc[eb][0:mj, :],
                                 scalar=iota_p2[0:mj, j:j + 1], in1=gbc[eb][0:mj, :], op0=ALU.is_equal, op1=ALU.mult)
                        for c in range(16):
                            hbk = xgi % 2
                            xgi += 1
                            gp = PS[hbk][:, 0:CAP]
                            for i in range(NT):
                                S.op('pe', 'matmul', [('h1b', i), ('selE', eb, i)], [('psg', hbk)], gp, lhsT=h1b[:, i, c * 128:(c + 1) * 128], rhs=selE[eb][:, i, :], start=(i == 0), stop=(i == NT - 1))
                            S.op('act', 'activation', [('psg', hbk)], [('xg', c)], out=xg[:, c, :], in_=gp, func=AF.Copy)
                        xgk = [('xg', c) for c in range(16)]
                        w_gu_v = w_gu[e].rearrange("(c p) f -> p c f", p=128)
                        for q in range(8):
                            s1 = wi % RW
                            s2 = (wi + 1) % RW
                            wi += 2
                            S.dma('pool', [], [('wb', s1)], out=wb[s1][:], in_=w_gu_v[:, :, q * 256:(q + 1) * 256])
                            S.dma('pool', [], [('wb', s2)], out=wb[s2][:], in_=w_gu_v[:, :, DM + q * 256:DM + (q + 1) * 256])
                            for j in range(2):
                                fc = q * 2 + j
                                pb = 2 + pri % 2
                                pri += 1
                                tb = pri % 2
                                gps = PS[pb][:, 0:CAP]
                                ups = PS[pb][:, 256:256 + CAP]
                                for c in range(16):
                                    S.op('pe', 'matmul', [('wb', s1)] + xgk, [('psgu', pb, 0)], gps, lhsT=wb[s1][:, c, j * 128:(j + 1) * 128], rhs=xg[:, c, :], start=(c == 0), stop=(c == 15))
                                for c in range(16):
                                    S.op('pe', 'matmul', [('wb', s2)] + xgk, [('psgu', pb, 1)], ups, lhsT=wb[s2][:, c, j * 128:(j + 1) * 128], rhs=xg[:, c, :], start=(c == 0), stop=(c == 15))
                                bg = bgs[:, e * 32 + fc:e * 32 + fc + 1]
                                bu = bgs[:, e * 32 + 16 + fc:e * 32 + 16 + fc + 1]
                                S.op('dve', 'tensor_scalar', [('psgu', pb, 0), ('psgu', pb, 1), 'bgs'], [('g1', tb)], out=g1[tb][:], in0=gps, scalar1=bg, scalar2=7.0, op0=ALU.add, op1=ALU.min)
                                S.op('act', 'activation', [('g1', tb)], [('sgm', tb)], out=sgm[tb][:], in_=g1[tb][:], func=AF.Sigmoid, scale=1.702)
                                S.op('dve', 'tensor_scalar', [('psgu', pb, 1), 'bgs'], [('u1', tb)], out=u1[tb][:], in0=ups, scalar1=bu, scalar2=7.0, op0=ALU.add, op1=ALU.min)
                                S.op('dve', 'tensor_scalar', [('u1', tb)], [('u1', tb)], out=u1[tb][:], in0=u1[tb][:], scalar1=-7.0, scalar2=1.0, op0=ALU.max, op1=ALU.add)
                                if pend is not None:
                                    ptb, pfc = pend
                                    S.op('dve', 'tensor_tensor', [('g1', ptb), ('sgm', ptb)], [('g1', ptb)], out=g1[ptb][:], in0=g1[ptb][:], in1=sgm[ptb][:], op=ALU.mult)
                                    S.op('dve', 'tensor_tensor', [('g1', ptb), ('u1', ptb)], [('actT', pfc)], out=actT[:, pfc, :], in0=g1[ptb][:], in1=u1[ptb][:], op=ALU.mult)
                                pend = (tb, fc)
                        ptb, pfc = pend
                        S.op('dve', 'tensor_tensor', [('g1', ptb), ('sgm', ptb)], [('g1', ptb)], out=g1[ptb][:], in0=g1[ptb][:], in1=sgm[ptb][:], op=ALU.mult)
                        S.op('dve', 'tensor_tensor', [('g1', ptb), ('u1', ptb)], [('actT', pfc)], out=actT[:, pfc, :], in0=g1[ptb][:], in1=u1[ptb][:], op=ALU.mult)
                        pend = None
                        ak = [('actT', fc) for fc in range(16)]
                        w_dn_v = w_dn[e].rearrange("(c p) f -> p c f", p=128)
                        for r in range(8):
                            db = dni % 2
                            dni += 1
                            s3 = wi % RW
                            wi += 1
                            S.dma('pool', [], [('wb', s3)], out=wb[s3][:], in_=w_dn_v[:, :, r * 256:(r + 1) * 256])
                            for j in range(2):
                                mj = MJ[j]
                                yp = PS[4 + j][0:mj, 0:256]
                                for c in range(16):
                                    S.op('pe', 'matmul', [('wb', s3)] + ak, [('psy', j)], yp, lhsT=actT[:, c, j * 128:j * 128 + mj], rhs=wb[s3][:, c, :], start=(c == 0), stop=(c == 15))
                                S.op('act', 'activation', [('psy', j)], [('yb', db, j)], out=yb[db][0:mj, j, :], in_=yp, func=AF.Copy)
                            for i in range(NT):
                                sp_ = PS[6 + i % 2][:, 0:256]
                                for j in range(2):
                                    S.op('pe', 'matmul', [('selG', eb, j), ('yb', db, j)], [('pss', i % 2)], sp_, lhsT=selG[eb][0:MJ[j], j, i * 128:(i + 1) * 128], rhs=yb[db][0:MJ[j], j, :], start=(j == 0), stop=(j == 1))
                                S.op('dve', 'tensor_tensor', [('pss', i % 2), ('acc', i, r)], [('acc', i, r)], out=acc[:, i, r * 256:(r + 1) * 256], in0=sp_, in1=acc[:, i, r * 256:(r + 1) * 256], op=ALU.add)
                    S.barrier()
                    if stop == 7:
                        dump_f(accraw[:].bitcast(F32), 0, 16384)
                        raise _Stop()

                with ExitStack() as se:
                    gb2 = sb(se, "gb2", [128, 2, DM])
                    stats = sb(se, "stats2", [128, 4, 6])
                    mv = sb(se, "mv2", [128, 2])
                    rs = sb(se, "rs2", [128, 1])
                    tmp = sb(se, "tmp2", [128, DM])
                    ob_ = [sb(se, "ob%d" % i, [128, DM]) for i in range(2)]
                    S.dma('sp', [], ['ln2gb'], out=gb2[:].rearrange("p a d -> p (a d)"), in_=ln2.rearrange("a d -> (a d)").partition_broadcast(128))
                    for i in range(NT):
                        fb = i % 2
                        layer_norm_tile(acc[:, i, :], ob_[fb][:], gb2, stats, mv, rs, tmp, [('acc', i)], [('ob', fb)], 'ln2')
                        S.dma('sp', [('ob', fb)], [('out', i)], out=out[i * 128:(i + 1) * 128, :], in_=ob_[fb][:])
        except _Stop:
            pass
        S.finalize(st)
    return nc


_CACHE = {}


def _consts():
    p = np.arange(128)
    cst = np.zeros((128, 1024), np.float32)
    cst[:, 0:128] = np.eye(128)
    cst[:, 128:256] = (p[:, None] <= p[None, :])
    cst[:, 256:384] = (p[:, None] < p[None, :])
    cst[:, 384:512] = 1.0
    cst[:, 512:768] = np.arange(256)[None, :]
    c2 = np.zeros((128, 64), np.float32)
    c2[:, 0] = p
    c2[:, 1] = p + 128
    for h in range(NH):
        for kk in range(2):
            c2[:, 2 + 2 * h + kk] = SLOPES[h] * (p + 128 * kk - 128)
    for i in range(NT):
        c2[:, 18 + i] = 128 - (i % 2) * 128 - p
    return cst, c2


def kernel(x, w_in, conv_w, conv_b, conv_ln_g, conv_ln_b, w_out, ln1_g, ln1_b,
           w_router, b_router, w_gate_up, b_gate_up, w_down, b_down, ln2_g, ln2_b):
    f = lambda a: np.ascontiguousarray(np.asarray(a, dtype=np.float32))
    x2 = f(x)[0]
    xT = np.ascontiguousarray(x2.T)
    if 'nc' not in _CACHE:
        _CACHE['nc'] = build()
    nc = _CACHE['nc']
    cst, c2 = _consts()
    convw = np.ascontiguousarray(f(conv_w).T.reshape(8, 128, 31).transpose(1, 0, 2).reshape(128, 8 * 31))
    cv = np.concatenate([f(v).reshape(8, 128).T for v in (conv_b, conv_ln_g, conv_ln_b)], axis=1)
    bgu = np.ascontiguousarray(f(b_gate_up).reshape(NE, 32, 128).transpose(2, 0, 1).reshape(128, NE * 32))
    shared = dict(
        xT=xT, w_in=f(w_in), convw=convw, convv=np.ascontiguousarray(cv), w_out=f(w_out),
        ln1=np.stack([f(ln1_g), f(ln1_b)]), ln2=np.stack([f(ln2_g), f(ln2_b)]),
        w_r=f(w_router), b_r=f(b_router).reshape(1, NE), w_gu=f(w_gate_up), bgu=bgu,
        w_dn=f(w_down), b_dn=f(b_down), cst=cst, cst2=c2)
    in_maps = []
    p = np.arange(128)
    for c in range(NCORES):
        t0 = c * TOK
        xto = np.zeros((DM, 1056), np.float32)
        lo = max(t0 - 32, 0)
        xto[:, 1056 - (t0 + TOK - lo):] = xT[:, lo:t0 + TOK]
        tq = t0 + np.arange(NT)[None, :, None] * 128 + p[:, None, None]
        n = np.arange(32)[None, None, :]
        past = n < (tq // 256)
        dt = np.where(past, -(tq - 256 * n - 128), 0).astype(np.float32).reshape(128, 256)
        fp = np.where(past, 0.0, NEG).astype(np.float32).reshape(128, 256)
        m = dict(shared)
        m.update(xTo=xto, xo=np.ascontiguousarray(x2[t0:t0 + TOK]), dtab=np.ascontiguousarray(dt), fptab=np.ascontiguousarray(fp))
        in_maps.append(m)
    res = run_bass_kernel_spmd(nc, in_maps, core_ids=list(range(NCORES)))
    outp = np.concatenate([r["out"] for r in res.results], axis=0)
    return outp.reshape(1, SEQ, DM).astype(np.float32)
```

```python
import numpy as np
from contextlib import ExitStack
import concourse.bass as bass
import concourse.mybir as mybir
from concourse.bass_utils import run_bass_kernel_spmd

F32 = mybir.dt.float32
BF16 = mybir.dt.bfloat16
AF = mybir.ActivationFunctionType
ALU = mybir.AluOpType
AX = mybir.AxisListType

NCORES = 8
SEQ = 8192
DM = 2048
TOK = 1024
NT = 8
NH = 8
NE = 32
CAP = 192
MJ = (128, CAP - 128)
ALPHA = 2.0 ** 0.25
SCALE = 128.0 ** -0.5
SLOPES = [2.0 ** (-(h + 1)) for h in range(NH)]
EPS = 1e-5
NEG = -1.0e30


class _Stop(Exception):
    pass


class _NoExc:
    def __init__(self, cm):
        self.cm = cm

    def __enter__(self):
        return self.cm.__enter__()

    def __exit__(self, *a):
        self.cm.__exit__(None, None, None)
        return False


class Sched:
    ENGS = ('pe', 'act', 'dve', 'pool', 'sp')
    NS = 8

    def __init__(self, nc):
        self.nc = nc
        self.streams = {e: [] for e in self.ENGS}
        self.lastw = {}
        self.readers = {}
        self.dma_since = []

    def _add(self, eng, fn, r, w, dma):
        idx = len(self.streams[eng])
        node = (eng, idx)
        deps = set()
        for k in r:
            lw = self.lastw.get(k)
            if lw is not None:
                deps.add(lw)
        for k in w:
            lw = self.lastw.get(k)
            if lw is not None and (lw[0] != eng or self.streams[lw[0]][lw[1]]['dma'] or dma):
                deps.add(lw)
            for rd in self.readers.get(k, ()):
                if rd[0] != eng or self.streams[rd[0]][rd[1]]['dma'] or dma:
                    deps.add(rd)
        deps.discard(node)
        rec = dict(fn=fn, deps=deps, dma=dma, has_dep=False)
        self.streams[eng].append(rec)
        for d in deps:
            self.streams[d[0]][d[1]]['has_dep'] = True
        for k in w:
            self.lastw[k] = node
            self.readers[k] = []
        for k in r:
            lst = self.readers.setdefault(k, [])
            if not dma:
                for j in range(len(lst)):
                    if lst[j][0] == eng and not self.streams[eng][lst[j][1]]['dma']:
                        lst[j] = node
                        break
                else:
                    lst.append(node)
            else:
                lst.append(node)
        if dma:
            self.dma_since.append(node)
        return node

    def op(self, eng, meth, r, w, *a, **k):
        return self._add(eng, lambda e: getattr(e, meth)(*a, **k), tuple(r), tuple(w), False)

    def dma(self, q, r, w, **k):
        return self._add(q, lambda e: e.dma_start(**k), tuple(r), tuple(w), True)

    def barrier(self):
        deps = set(self.dma_since)
        for e in self.ENGS:
            st = self.streams[e]
            for i in range(len(st) - 1, -1, -1):
                if st[i]['fn'] is not None and not st[i]['dma']:
                    deps.add((e, i))
                    break
        for d in deps:
            self.streams[d[0]][d[1]]['has_dep'] = True
        for e in self.ENGS:
            self.streams[e].append(dict(fn=None, deps=set(deps), dma=False, has_dep=False))
        self.dma_since = []
        self.lastw = {}
        self.readers = {}

    def finalize(self, stack):
        nc = self.nc
        esem = {e: stack.enter_context(nc.semaphore("es_" + e)) for e in self.ENGS}
        dsem = {e: [stack.enter_context(nc.semaphore("ds_%s%d" % (e, i))) for i in range(self.NS)]
                for e in ('sp', 'act', 'pool')}
        ndma = {}
        for e in self.ENGS:
            tick = 0
            nd = 0
            for rec in self.streams[e]:
                if rec['fn'] is None:
                    continue
                if rec['dma']:
                    s = dsem[e][nd % self.NS]
                    rec['tok'] = (s, 16 * (nd // self.NS + 1))
                    rec['pre'] = (s, 16 * (nd // self.NS))
                    nd += 1
                elif rec['has_dep']:
                    tick += 1
                    rec['tok'] = (esem[e], tick)
            ndma[e] = nd
        block = stack.enter_context(nc.Block())
        streams = self.streams
        NS = self.NS

        def emit(e, engobj):
            waited = {}

            def wait(tok):
                s, v = tok
                if v <= 0:
                    return
                key = id(s)
                if waited.get(key, (None, 0))[1] >= v:
                    return
                waited[key] = (s, v)
                engobj.wait_ge(s, v)
            for rec in streams[e]:
                for d in sorted(rec['deps']):
                    if d[0] == e and not streams[d[0]][d[1]]['dma'] and rec['fn'] is None:
                        continue
                    wait(streams[d[0]][d[1]]['tok'])
                if rec['fn'] is None:
                    continue
                if rec['dma']:
                    wait(rec['pre'])
                    rec['fn'](engobj).then_inc(rec['tok'][0], 16)
                else:
                    ins = rec['fn'](engobj)
                    if rec['has_dep']:
                        ins.then_inc(rec['tok'][0], 1)
            if e == 'sp':
                for q in ('sp', 'act', 'pool'):
                    nd = ndma[q]
                    for i in range(min(nd, NS)):
                        cnt = (nd - 1 - i) // NS + 1
                        wait((dsem[q][i], 16 * cnt))

        @block.tensor
        def _(eng):
            emit('pe', eng)

        @block.scalar
        def _(eng):
            emit('act', eng)

        @block.vector
        def _(eng):
            emit('dve', eng)

        @block.gpsimd
        def _(eng):
            emit('pool', eng)

        @block.sync
        def _(eng):
            emit('sp', eng)


def build(stop=99):
    nc = bass.Bass("TRN2", target_bir_lowering=False)

    def din(name, shape, dt=F32):
        return nc.dram_tensor(name, list(shape), dt, kind="ExternalInput").ap()

    xT = din("xT", [DM, SEQ])
    xTo = din("xTo", [DM, 1056])
    xo = din("xo", [TOK, DM])
    w_in = din("w_in", [DM, 5120])
    convw = din("convw", [128, 8 * 31])
    convv = din("convv", [128, 24])
    w_out = din("w_out", [DM, DM])
    ln1 = din("ln1", [2, DM])
    ln2 = din("ln2", [2, DM])
    w_r = din("w_r", [DM, NE])
    b_r = din("b_r", [1, NE])
    w_gu = din("w_gu", [NE, DM, 2 * DM]) if stop >= 7 else None
    bgu = din("bgu", [128, NE * 32])
    w_dn = din("w_dn", [NE, DM, DM]) if stop >= 7 else None
    b_dn = din("b_dn", [NE, DM])
    cst = din("cst", [128, 1024])
    cst2 = din("cst2", [128, 64])
    dtab = din("dtab", [128, 8 * 32])
    fptab = din("fptab", [128, 8 * 32])
    out = nc.dram_tensor("out", [TOK, DM], F32, kind="ExternalOutput").ap()
    if stop < 99:
        dbgf = nc.dram_tensor("dbgf", [128, 16384], F32, kind="ExternalOutput").ap()
        dbgb = nc.dram_tensor("dbgb", [128, 16384], BF16, kind="ExternalOutput").ap()
    KTs = nc.dram_tensor("KTs", [NH, 128, SEQ], BF16, kind="Internal").ap()
    Vs = nc.dram_tensor("Vs", [SEQ, 1024], BF16, kind="Internal").ap()
    RK = nc.dram_tensor("RK", [NE, TOK], F32, kind="Internal").ap()
    GT = nc.dram_tensor("GT", [NE, TOK], F32, kind="Internal").ap()

    with ExitStack() as st:
        S = Sched(nc)

        def dump_f(ap, off, n):
            S.dma('sp', [], [('dbgf', off)], out=dbgf[:, off:off + n], in_=ap)

        def sb(stack, name, shape, dt=F32):
            return stack.enter_context(_NoExc(nc.sbuf_tensor(name, list(shape), dt)))

        PS = [st.enter_context(nc.psum_tensor("ps%d" % i, [128, 512], F32)) for i in range(8)]

        cf = sb(st, "cf", [128, 1024])
        cb = sb(st, "cb", [128, 512], BF16)
        c2 = sb(st, "c2", [128, 64])
        S.dma('sp', [], ['cf'], out=cf[:], in_=cst[:, :])
        S.dma('sp', [], ['c2'], out=c2[:], in_=cst2[:, :])
        S.op('dve', 'tensor_copy', ['cf'], ['cb'], out=cb[:], in_=cf[:, 0:512])
        identf = cf[:, 0:128]
        onesf = cf[:, 384:512]
        iota_row = cf[:, 512:768]
        identb = cb[:, 0:128]
        trib = cb[:, 128:256]
        utrib = cb[:, 256:384]
        onesb = cb[:, 384:512]
        iota_p2 = c2[:, 0:2]
        kbias = c2[:, 2:18]
        down = c2[:, 18:26]

        accraw = sb(st, "accraw", [128, 2 * NT * DM], BF16)
        acc = accraw[:].bitcast(F32).rearrange("p (a d) -> p a d", d=DM)
        S.barrier()

        try:
            with ExitStack() as s1:
                convT = sb(s1, "convT", [128, 8, TOK], BF16)
                kmean = sb(s1, "kmean", [128, NH, 32])
                w_in_v = w_in.rearrange("(c p) f -> p c f", p=128)
                accf = accraw[:].bitcast(F32)
                accb = accraw[:]

                with ExitStack() as sc:
                    wga = [sb(sc, "wga%d" % i, [128, 16, 128], BF16) for i in range(2)]
                    wgb = [sb(sc, "wgb%d" % i, [128, 16, 128], BF16) for i in range(2)]
                    cw = sb(sc, "cw", [128, 8, 31])
                    cv = sb(sc, "cv", [128, 24])
                    sg = [sb(sc, "sg%d" % i, [128, 1056]) for i in range(2)]
                    ug = [sb(sc, "ug%d" % i, [128, 1056]) for i in range(2)]
                    sq = [sb(sc, "sq%d" % i, [128, 1024]) for i in range(2)]
                    xto = sb(sc, "xto_c", [128, 16, 1056], BF16)
                    S.dma('pool', [], ['xto'], out=xto[:], in_=xTo.rearrange("(c p) t -> p c t", p=128))
                    cT = accf[:, 0:8192].rearrange("p (g t) -> p g t", t=TOK)
                    ty = [accf[:, 8192 + i * 1024:8192 + (i + 1) * 1024] for i in range(2)]
                    tz = [accf[:, 10240 + i * 1024:10240 + (i + 1) * 1024] for i in range(2)]
                    mean = accf[:, 12288:13312]
                    rstd = accf[:, 13312:14336]
                    msq = accf[:, 14336:15360]
                    S.dma('sp', [], ['cw'], out=cw[:], in_=convw.rearrange("p (g j) -> p g j", j=31))
                    S.dma('sp', [], ['cv'], out=cv[:], in_=convv[:, :])
                    for g in range(8):
                        b = g % 2
                        S.dma('pool', [], [('wga', b)], out=wga[b][:], in_=w_in_v[:, :, 3072 + g * 128:3072 + (g + 1) * 128])
                        S.dma('pool', [], [('wgb', b)], out=wgb[b][:], in_=w_in_v[:, :, 4096 + g * 128:4096 + (g + 1) * 128])
                        for k in range(3):
                            pa = k % 2
                            pbb = 2 + k % 2
                            cs = slice(k * 352, (k + 1) * 352)
                            for c in range(16):
                                S.op('pe', 'matmul', [('wga', b), 'xto'], [('ps', pa)], PS[pa][:, 0:352],
                                     lhsT=wga[b][:, c, :], rhs=xto[:, c, cs], start=(c == 0), stop=(c == 15))
                            for c in range(16):
                                S.op('pe', 'matmul', [('wgb', b), 'xto'], [('ps', pbb)], PS[pbb][:, 0:352],
                                     lhsT=wgb[b][:, c, :], rhs=xto[:, c, cs], start=(c == 0), stop=(c == 15))
                            S.op('act', 'activation', [('ps', pbb)], [('sg', b, k)], out=sg[b][:, cs], in_=PS[pbb][:, 0:352], func=AF.Sigmoid)
                            S.op('dve', 'tensor_tensor', [('ps', pa), ('sg', b, k)], [('ug', b, k)], out=ug[b][:, cs], in0=PS[pa][:, 0:352], in1=sg[b][:, cs], op=ALU.mult)
                        ukeys = [('ug', b, k) for k in range(3)]
                        S.op('dve', 'tensor_scalar', ukeys + ['cw', 'cv'], [('cT', g)], out=cT[:, g, :], in0=ug[b][:, 2:2 + TOK],
                             scalar1=cw[:, g, 0:1], scalar2=cv[:, g:g + 1], op0=ALU.mult, op1=ALU.add)
                        S.op('dve', 'tensor_scalar', ukeys + ['cw'], [('ty', b)], out=ty[b], in0=ug[b][:, 3:3 + TOK],
                             scalar1=cw[:, g, 1:2], scalar2=None, op0=ALU.mult)
                        for j in range(2, 31):
                            if j % 2 == 0:
                                S.op('dve', 'scalar_tensor_tensor', ukeys + ['cw', ('cT', g)], [('cT', g)], out=cT[:, g, :], in0=ug[b][:, j + 2:j + 2 + TOK],
                                     scalar=cw[:, g, j:j + 1], in1=cT[:, g, :], op0=ALU.mult, op1=ALU.add)
                            else:
                                S.op('dve', 'scalar_tensor_tensor', ukeys + ['cw', ('ty', b)], [('ty', b)], out=ty[b], in0=ug[b][:, j + 2:j + 2 + TOK],
                                     scalar=cw[:, g, j:j + 1], in1=ty[b], op0=ALU.mult, op1=ALU.add)
                        S.op('dve', 'tensor_tensor', [('cT', g), ('ty', b)], [('cT', g)], out=cT[:, g, :], in0=cT[:, g, :], in1=ty[b], op=ALU.add)
                        S.op('act', 'activation', [('cT', g)], [('sq', b)], out=sq[b][:], in_=cT[:, g, :], func=AF.Square)
                        for half in range(2):
                            hs = slice(half * 512, (half + 1) * 512)
                            S.op('pe', 'matmul', [('cT', g)], [('ps', 4 + half)], PS[4 + half][:], lhsT=onesf, rhs=cT[:, g, hs], start=(g == 0), stop=(g == 7))
                            S.op('pe', 'matmul', [('sq', b)], [('ps', 6 + half)], PS[6 + half][:], lhsT=onesf, rhs=sq[b][:, hs], start=(g == 0), stop=(g == 7))
                    for half in range(2):
                        hs = slice(half * 512, (half + 1) * 512)
                        S.op('act', 'activation', [('ps', 4 + half)], [('mean', half)], out=mean[:, hs], in_=PS[4 + half][:], func=AF.Copy, scale=1.0 / 1024.0)
                        S.op('dve', 'tensor_tensor', [('mean', half)], [('msq', half)], out=msq[:, hs], in0=mean[:, hs], in1=mean[:, hs], op=ALU.mult)
                        S.op('dve', 'scalar_tensor_tensor', [('ps', 6 + half), ('msq', half)], [('rstd', half)], out=rstd[:, hs], in0=PS[6 + half][:],
                             scalar=1.0 / 1024.0, in1=msq[:, hs], op0=ALU.mult, op1=ALU.subtract)
                        S.op('dve', 'tensor_scalar', [('rstd', half)], [('rstd', half)], out=rstd[:, hs], in0=rstd[:, hs], scalar1=EPS, scalar2=None, op0=ALU.add)
                        S.op('act', 'activation', [('rstd', half)], [('rstd', half)], out=rstd[:, hs], in_=rstd[:, hs], func=AF.Sqrt)
                        S.op('dve', 'reciprocal', [('rstd', half)], [('rstd', half)], out=rstd[:, hs], in_=rstd[:, hs])
                    for g in range(8):
                        b = g % 2
                        S.op('dve', 'tensor_tensor', [('cT', g), ('mean', 0), ('mean', 1)], [('ty', b)], out=ty[b][:], in0=cT[:, g, :], in1=mean[:], op=ALU.subtract)
                        S.op('dve', 'tensor_tensor', [('ty', b), ('rstd', 0), ('rstd', 1)], [('ty', b)], out=ty[b][:], in0=ty[b][:], in1=rstd[:], op=ALU.mult)
                        S.op('act', 'activation', [('ty', b), 'cv'], [('tz', b)], out=tz[b][:], in_=ty[b][:], func=AF.Identity, scale=cv[:, 8 + g:9 + g], bias=cv[:, 16 + g:17 + g])
                        S.op('act', 'activation', [('tz', b)], [('ty', b)], out=ty[b][:], in_=tz[b][:], func=AF.Sigmoid)
                        S.op('dve', 'tensor_tensor', [('ty', b), ('tz', b)], [('convT', g)], out=convT[:, g, :], in0=tz[b][:], in1=ty[b][:], op=ALU.mult)
                    S.barrier()
                    if stop == 1:
                        S.dma('sp', [], ['dbgb1'], out=dbgb[:, 0:8192], in_=convT[:].rearrange('p a d -> p (a d)'))
                        raise _Stop()

                with ExitStack() as sa:
                    Wk = accb[:, 0:16384].rearrange("p (c f) -> p c f", f=1024)
                    Wv = accb[:, 16384:32768].rearrange("p (c f) -> p c f", f=1024)
                    for cg in range(4):
                        S.dma('pool', [], [('Wk', cg)], out=Wk[:, cg * 4:(cg + 1) * 4, :], in_=w_in_v[:, cg * 4:(cg + 1) * 4, 1024:2048])
                        S.dma('pool', [], [('Wv', cg)], out=Wv[:, cg * 4:(cg + 1) * 4, :], in_=w_in_v[:, cg * 4:(cg + 1) * 4, 2048:3072])
                    with ExitStack() as sa1:
                        xc = [sb(sa1, "xc%d" % i, [128, 16, 512], BF16) for i in range(2)]
                        ktsb = [sb(sa1, "ktsb%d" % i, [128, 512], BF16) for i in range(3)]
                        vsb = [sb(sa1, "vsb%d" % i, [128, 1024], BF16) for i in range(2)]
                        kmsum = sb(sa1, "kmsum", [128, NH, 32])
                        xT_v = xT.rearrange("(c p) t -> p c t", p=128)
                        ke = 0
                        ve = 0
                        for tcn in range(16):
                            b = tcn % 2
                            S.dma('pool', [], [('xc', b)], out=xc[b][:], in_=xT_v[:, :, tcn * 512:(tcn + 1) * 512])
                            for h in range(NH):
                                pb = h % 2
                                for c in range(16):
                                    S.op('pe', 'matmul', [('Wk', c // 4), ('xc', b)], [('ps', pb)], PS[pb][:],
                                         lhsT=Wk[:, c, h * 128:(h + 1) * 128], rhs=xc[b][:, c, :],
                                         start=(c == 0), stop=(c == 15))
                                kb = ke % 3
                                ke += 1
                                S.op('act', 'activation', [('ps', pb)], [('ktsb', kb)], out=ktsb[kb][:], in_=PS[pb][:], func=AF.Copy)
                                if True:
                                  S.op('dve', 'tensor_reduce', [('ktsb', kb)], [('kmsum', h, tcn)],
                                     out=kmsum[:, h, 2 * tcn:2 * tcn + 2],
                                     in_=ktsb[kb][:].rearrange("p (b t) -> p b t", t=256), axis=AX.X, op=ALU.add)
                                if True:
                                  S.dma('sp', [('ktsb', kb)], [('KTs', h, tcn)], out=KTs[h, :, tcn * 512:(tcn + 1) * 512], in_=ktsb[kb][:])
                            for s in range(4):
                                vb = ve % 2
                                ve += 1
                                for half in range(2):
                                    pb = 2 + half
                                    for c in range(16):
                                        S.op('pe', 'matmul', [('Wv', c // 4), ('xc', b)], [('ps', pb)], PS[pb][:],
                                             lhsT=xc[b][:, c, s * 128:(s + 1) * 128], rhs=Wv[:, c, half * 512:(half + 1) * 512],
                                             start=(c == 0), stop=(c == 15))
                                    if half == 0:
                                        S.op('act', 'activation', [('ps', pb)], [('vsb', vb, half)], out=vsb[vb][:, 0:512], in_=PS[pb][:], func=AF.Copy)
                                    else:
                                        S.op('dve', 'tensor_copy', [('ps', pb)], [('vsb', vb, half)], out=vsb[vb][:, 512:1024], in_=PS[pb][:])
                                t0 = tcn * 512 + s * 128
                                if True:
                                  S.dma('sp', [('vsb', vb, 0), ('vsb', vb, 1)], [('Vs', t0)], out=Vs[t0:t0 + 128, :], in_=vsb[vb][:])
                        S.op('dve', 'tensor_scalar', [('kmsum', h, t) for h in range(NH) for t in range(16)], ['kmean'],
                             out=kmean[:], in0=kmsum[:], scalar1=1.0 / 256.0, scalar2=None, op0=ALU.mult)
                        S.barrier()
                        if stop == 2:
                            dump_f(kmean[:].rearrange('p a b -> p (a b)'), 2048, 256)
                            raise _Stop()

                    QT = sb(s1, "QT", [128, NH, TOK], BF16)
                    KTo = sb(s1, "KTo", [128, NH, TOK], BF16)
                    Vo = sb(s1, "Vo", [128, NT, NH, 129], BF16)
                    Wt = sb(s1, "Wt", [128, NH, NT, 32])
                    with ExitStack() as sa2:
                        xto = sb(sa2, "xto_a", [128, 16, 1056], BF16)
                        S.dma('pool', [], ['xto'], out=xto[:], in_=xTo.rearrange("(c p) t -> p c t", p=128))
                        Wqh = [sb(sa2, "Wq%d" % i, [128, 16, 128], BF16) for i in range(2)]
                        qf = [sb(sa2, "qf%d" % i, [128, 512]) for i in range(2)]
                        dt_sb = sb(sa2, "dt_sb", [128, NT, 32])
                        fp_sb = sb(sa2, "fp_sb", [128, NT, 32])
                        gm = [sb(sa2, "gm%d" % i, [128, 32]) for i in range(2)]
                        t8 = [sb(sa2, "t8%d" % i, [128, 8]) for i in range(2)]
                        thr = [sb(sa2, "thr%d" % i, [128, 1]) for i in range(2)]
                        sel = [sb(sa2, "sel%d" % i, [128, 32]) for i in range(2)]
                        ex = [sb(sa2, "ex%d" % i, [128, 32]) for i in range(2)]
                        S.dma('sp', [], ['dt_sb'], out=dt_sb[:], in_=dtab.rearrange("p (i n) -> p i n", n=32))
                        S.dma('sp', [], ['fp_sb'], out=fp_sb[:], in_=fptab.rearrange("p (i n) -> p i n", n=32))
                        S.op('pool', 'memset', [], [('Vo1',)], Vo[:, :, :, 128:129], 1.0)
                        cnt = 0
                        for h in range(NH):
                            S.dma('pool', [], [('Wq', h % 2)], out=Wqh[h % 2][:], in_=w_in_v[:, :, h * 128:(h + 1) * 128])
                            for qc in range(2):
                                pb = qc
                                for c in range(16):
                                    S.op('pe', 'matmul', [('Wq', h % 2), 'xto'], [('ps', pb)], PS[pb][:],
                                         lhsT=Wqh[h % 2][:, c, :], rhs=xto[:, c, 32 + qc * 512:32 + (qc + 1) * 512],
                                         start=(c == 0), stop=(c == 15))
                                S.op('dve', 'tensor_copy', [('ps', pb)], [('qf', qc)], out=qf[qc][:], in_=PS[pb][:])
                                S.op('act', 'activation', [('qf', qc)], [('QT', h, qc)], out=QT[:, h, qc * 512:(qc + 1) * 512], in_=qf[qc][:], func=AF.Copy)
                                for j in range(4):
                                    i = qc * 4 + j
                                    k2 = cnt % 2
                                    cnt += 1
                                    gp = PS[4 + k2][:, 0:32]
                                    S.op('pe', 'matmul', [('qf', qc), 'kmean'], [('ps', 4 + k2)], gp,
                                         lhsT=qf[qc][:, j * 128:(j + 1) * 128], rhs=kmean[:, h, :], start=True, stop=True)
                                    S.op('dve', 'tensor_tensor', [('ps', 4 + k2), 'fp_sb'], [('gm', k2)], out=gm[k2][:], in0=gp, in1=fp_sb[:, i, :], op=ALU.add)
                                    S.op('dve', 'max', [('gm', k2)], [('t8', k2)], out=t8[k2][:], in_=gm[k2][:])
                                    S.op('dve', 'tensor_scalar', [('t8', k2)], [('thr', k2)], out=thr[k2][:], in0=t8[k2][:, 2:3], scalar1=-1.0e29, scalar2=None, op0=ALU.max)
                                    S.op('dve', 'tensor_scalar', [('gm', k2), ('thr', k2)], [('sel', k2)], out=sel[k2][:], in0=gm[k2][:], scalar1=thr[k2][:, 0:1], scalar2=None, op0=ALU.is_ge)
                                    S.op('act', 'activation', ['dt_sb'], [('ex', k2)], out=ex[k2][:], in_=dt_sb[:, i, :], func=AF.Exp, scale=SLOPES[h])
                                    S.op('dve', 'tensor_tensor', [('sel', k2), ('ex', k2)], [('Wt', h, i)], out=Wt[:, h, i, :], in0=sel[k2][:], in1=ex[k2][:], op=ALU.mult)
                            for qc in range(2):
                                pb = 2 + qc
                                for c in range(16):
                                    S.op('pe', 'matmul', ['Wk', 'xto'], [('ps', pb)], PS[pb][:],
                                         lhsT=Wk[:, c, h * 128:(h + 1) * 128], rhs=xto[:, c, 32 + qc * 512:32 + (qc + 1) * 512],
                                         start=(c == 0), stop=(c == 15))
                                S.op('act', 'activation', [('ps', pb)], [('KTo', h, qc)], out=KTo[:, h, qc * 512:(qc + 1) * 512], in_=PS[pb][:], func=AF.Copy)
                        for i in range(NT):
                            for half in range(2):
                                pb = 6 + half
                                for c in range(16):
                                    S.op('pe', 'matmul', ['Wv', 'xto'], [('ps', pb)], PS[pb][:],
                                         lhsT=xto[:, c, 32 + i * 128:32 + (i + 1) * 128], rhs=Wv[:, c, half * 512:(half + 1) * 512],
                                         start=(c == 0), stop=(c == 15))
                                S.op('dve' if half else 'act', 'tensor_copy' if half else 'activation', [('ps', pb)], [('Vo', i, half)],
                                     **(dict(out=Vo[:, i, half * 4:(half + 1) * 4, 0:128], in_=PS[pb][:].rearrange("p (h d) -> p h d", d=128))
                                        if half else dict(out=Vo[:, i, 0:4, 0:128], in_=PS[pb][:].rearrange("p (h d) -> p h d", d=128), func=AF.Copy)))
                        S.barrier()
                        if stop == 3:
                            S.dma('sp', [], ['dbgb3'], out=dbgb[:, 0:8192], in_=QT[:].rearrange('p a d -> p (a d)'))
                            S.dma('sp', [], ['dbgb3b'], out=dbgb[:, 8192:16384], in_=KTo[:].rearrange('p a d -> p (a d)'))
                            dump_f(Wt[:].rearrange('p a b c -> p (a b c)'), 0, 2048)
                            dump_f(kmean[:].rearrange('p a b -> p (a b)'), 2048, 256)
                            raise _Stop()

                attnT = sb(s1, "attnT", [128, NH, TOK], BF16)
                with ExitStack() as sbk:
                    KTh = [accb[:, i * 8192:(i + 1) * 8192] for i in range(2)]
                    Vh = [accb[:, 16384:16384 + 64 * 129].rearrange("p (n d) -> p n d", d=129), sb(sbk, "Vh1", [128, 64, 129], BF16)]
                    PT = [sb(sbk, "PT%d" % i, [128, 512], BF16) for i in range(4)]
                    PO = [sb(sbk, "PO%d" % i, [128, 128], BF16) for i in range(2)]
                    ac = sb(sbk, "ac", [128, NT, 129])
                    wown = sb(sbk, "wown", [128, NT])
                    rec = sb(sbk, "rec", [128, NT])
                    atok = [sb(sbk, "atok%d" % i, [128, 128], BF16) for i in range(2)]
                    Vs_v = Vs.rearrange("(n p) d -> p n d", p=128)
                    for i in range(2):
                        S.op('pool', 'memset', [], [('Vh1', i)], Vh[i][:, :, 128:129], 1.0)
                    pti = 0
                    oi = 0
                    for h in range(NH):
                        hb = h % 2
                        S.dma('sp', [], [('KTh', hb)], out=KTh[hb][:], in_=KTs[h, :, :])
                        for vg in range(8):
                            S.dma('sp', [('Vh1', hb)], [('Vh', hb, vg)], out=Vh[hb][:, vg * 8:(vg + 1) * 8, 0:128], in_=Vs_v[:, vg * 8:(vg + 1) * 8, h * 128:(h + 1) * 128])
                        S.op('dve', 'memset', [], [('ac', i) for i in range(NT)], ac[:], 0.0)
                        S.op('act', 'activation', ['c2'], ['wown'], out=wown[:], in_=down, func=AF.Exp, scale=SLOPES[h])
                        steps = [(n, qc) for n in range(31) for qc in range(2)]

                        def emit_st(n, qc):
                            nonlocal pti
                            pts = []
                            for kk in range(2):
                                pb = pti % 4
                                pti += 1
                                S.op('pe', 'matmul', [('KTh', hb), ('QT', h, qc)], [('ps', pb)], PS[pb][:],
                                     lhsT=KTh[hb][:, (2 * n + kk) * 128:(2 * n + kk + 1) * 128], rhs=QT[:, h, qc * 512:(qc + 1) * 512], start=True, stop=True)
                                pts.append(pb)
                            return pts
                        nxt = emit_st(*steps[0])
                        for si, (n, qc) in enumerate(steps):
                            pts = nxt
                            for kk in range(2):
                                pb = pts[kk]
                                S.op('act', 'activation', [('ps', pb), 'c2'], [('PT', pb)], out=PT[pb][:], in_=PS[pb][:], func=AF.Exp,
                                     scale=SCALE, bias=kbias[:, 2 * h + kk:2 * h + kk + 1])
                            if si + 1 < len(steps):
                                nxt = emit_st(*steps[si + 1])
                            for qs in range(4):
                                i = qc * 4 + qs
                                ob = oi % 4
                                oi += 1
                                ops_ = PS[4 + ob][:, 0:129]
                                for kk in range(2):
                                    S.op('pe', 'matmul', [('PT', pts[kk]), ('Vh', hb, (2 * n + kk) // 8)], [('po', ob)], ops_,
                                         lhsT=PT[pts[kk]][:, qs * 128:(qs + 1) * 128], rhs=Vh[hb][:, 2 * n + kk, :], start=(kk == 0), stop=(kk == 1))
                                S.op('dve', 'scalar_tensor_tensor', [('po', ob), ('Wt', h, i), ('ac', i)], [('ac', i)], out=ac[:, i, :], in0=ops_,
                                     scalar=Wt[:, h, i, n:n + 1], in1=ac[:, i, :], op0=ALU.mult, op1=ALU.add)
                        for i in range(NT):
                            ob = oi % 4
                            oi += 1
                            ops_ = PS[4 + ob][:, 0:129]
                            nk = i % 2 + 1
                            for kk in range(nk):
                                pb = pti % 4
                                pti += 1
                                k0 = (i // 2) * 256 + kk * 128
                                S.op('pe', 'matmul', [('KTo', h, k0 // 512), ('QT', h, i // 4)], [('ps', pb)], PS[pb][:, 0:128],
                                     lhsT=KTo[:, h, k0:k0 + 128], rhs=QT[:, h, i * 128:(i + 1) * 128], start=True, stop=True)
                                po = (pti) % 2
                                S.op('act', 'activation', [('ps', pb), 'c2'], [('PO', po)], out=PO[po][:], in_=PS[pb][:, 0:128], func=AF.Exp,
                                     scale=SCALE, bias=kbias[:, 2 * h + kk:2 * h + kk + 1])
                                if kk == i % 2:
                                    S.op('dve', 'tensor_tensor', [('PO', po), 'cb'], [('PO', po)], out=PO[po][:], in0=PO[po][:], in1=trib, op=ALU.mult)
                                vt = (i // 2) * 2 + kk
                                S.op('pe', 'matmul', [('PO', po), ('Vo', vt, h // 4), ('Vo1',)], [('po', ob)], ops_,
                                     lhsT=PO[po][:], rhs=Vo[:, vt, h, :], start=(kk == 0), stop=(kk == nk - 1))
                            S.op('dve', 'scalar_tensor_tensor', [('po', ob), 'wown', ('ac', i)], [('ac', i)], out=ac[:, i, :], in0=ops_,
                                 scalar=wown[:, i:i + 1], in1=ac[:, i, :], op0=ALU.mult, op1=ALU.add)
                        S.op('dve', 'reciprocal', [('ac', i) for i in range(NT)], ['rec'], out=rec[:], in_=ac[:, :, 128])
                        for i in range(NT):
                            ab = i % 2
                            S.op('dve', 'tensor_scalar', [('ac', i), 'rec'], [('atok', ab)], out=atok[ab][:], in0=ac[:, i, 0:128], scalar1=rec[:, i:i + 1], scalar2=None, op0=ALU.mult)
                            pb = pti % 4
                            pti += 1
                            S.op('pe', 'matmul', [('atok', ab), 'cb'], [('ps', pb)], PS[pb][:, 0:128], lhsT=atok[ab][:], rhs=identb, start=True, stop=True)
                            S.op('act', 'activation', [('ps', pb)], [('attnT', h, i)], out=attnT[:, h, i * 128:(i + 1) * 128], in_=PS[pb][:, 0:128], func=AF.Copy)
                    S.barrier()
                    if stop == 4:
                        S.dma('sp', [], ['dbgb4'], out=dbgb[:, 0:8192], in_=attnT[:].rearrange('p a d -> p (a d)'))
                        raise _Stop()

                with ExitStack() as sc3:
                    wo = [sb(sc3, "wo%d" % i, [128, 16, 512], BF16) for i in range(2)]
                    xr = [sb(sc3, "xr%d" % i, [128, 512]) for i in range(3)]
                    w_out_v = w_out.rearrange("(c p) f -> p c f", p=128)
                    xi = 0
                    for cc in range(4):
                        b = cc % 2
                        S.dma('pool', [], [('wo', b)], out=wo[b][:], in_=w_out_v[:, :, cc * 512:(cc + 1) * 512])
                        for i in range(NT):
                            pb = i % 4
                            xb = xi % 3
                            xi += 1
                            S.dma('sp', [], [('xr', xb)], out=xr[xb][:], in_=xo[i * 128:(i + 1) * 128, cc * 512:(cc + 1) * 512])
                            for c in range(16):
                                src = attnT if c < 8 else convT
                                S.op('pe', 'matmul', [('wo', b)], [('ps', pb)], PS[pb][:],
                                     lhsT=src[:, c % 8, i * 128:(i + 1) * 128], rhs=wo[b][:, c, :], start=(c == 0), stop=(c == 15))
                            S.op('dve', 'scalar_tensor_tensor', [('ps', pb), ('xr', xb)], [('acc', i, cc)], out=acc[:, i, cc * 512:(cc + 1) * 512], in0=xr[xb][:],
                                 scalar=ALPHA, in1=PS[pb][:], op0=ALU.mult, op1=ALU.add)
                    S.barrier()
                    if stop == 5:
                        dump_f(accraw[:].bitcast(F32), 0, 16384)
                        raise _Stop()

            def layer_norm_tile(src_ap, dst_ap, gb, stats, mv, rs, tmp, keys_r, keys_w, tag):
                for k in range(4):
                    S.op('dve', 'bn_stats', keys_r, [(tag, 'st', k)], out=stats[:, k, :], in_=src_ap[:, k * 512:(k + 1) * 512])
                S.op('dve', 'bn_aggr', [(tag, 'st', k) for k in range(4)], [(tag, 'mv')], out=mv[:], in_=stats[:].rearrange("p a b -> p (a b)"))
                S.op('dve', 'tensor_scalar', [(tag, 'mv')], [(tag, 'rs')], out=rs[:], in0=mv[:, 1:2], scalar1=EPS, scalar2=None, op0=ALU.add)
                S.op('act', 'activation', [(tag, 'rs')], [(tag, 'rs')], out=rs[:], in_=rs[:], func=AF.Sqrt)
                S.op('dve', 'reciprocal', [(tag, 'rs')], [(tag, 'rs')], out=rs[:], in_=rs[:])
                S.op('dve', 'tensor_scalar', keys_r + [(tag, 'mv'), (tag, 'rs')], [(tag, 'tmp')], out=tmp[:], in0=src_ap, scalar1=mv[:, 0:1], scalar2=rs[:, 0:1],
                     op0=ALU.subtract, op1=ALU.mult)
                S.op('dve', 'tensor_tensor', [(tag, 'tmp'), tag + 'gb'], [(tag, 'tmp')], out=tmp[:], in0=tmp[:], in1=gb[:, 0, :], op=ALU.mult)
                S.op('dve', 'tensor_tensor', [(tag, 'tmp'), tag + 'gb'], keys_w, out=dst_ap, in0=tmp[:], in1=gb[:, 1, :], op=ALU.add)

            with ExitStack() as s2:
                h1b = sb(s2, "h1b", [128, NT, DM], BF16)
                gate = sb(s2, "gate", [128, NT, NE])
                rankm = sb(s2, "rankm", [128, NT, NE])
                gateT = sb(s2, "gateT", [NE, TOK])
                bgs = sb(s2, "bgs", [128, NE * 32])
                S.dma('sp', [], ['bgs'], out=bgs[:], in_=bgu[:, :])
                with ExitStack() as sr:
                    gb1 = sb(sr, "gb1", [128, 2, DM])
                    stats = sb(sr, "stats", [128, 4, 6])
                    mv = sb(sr, "mv", [128, 2])
                    rs = sb(sr, "rs", [128, 1])
                    tmp = sb(sr, "tmp", [128, DM])
                    h1f = [sb(sr, "h1f%d" % i, [128, DM]) for i in range(2)]
                    hT = [sb(sr, "hT%d" % i, [128, 128]) for i in range(4)]
                    wr = sb(sr, "wr", [128, 16, NE])
                    brb = sb(sr, "brb", [128, NE])
                    lg = sb(sr, "lg", [128, NE])
                    t8 = sb(sr, "t8r", [128, 8])
                    nmx = sb(sr, "nmx", [128, 1])
                    selt = sb(sr, "selt", [128, NT, NE])
                    selb = sb(sr, "selb", [128, NT, NE], BF16)
                    exr = sb(sr, "exr", [128, NE])
                    den = sb(sr, "den", [128, 1])
                    rkT = sb(sr, "rkT", [NE, TOK])
                    slT = sb(sr, "slT", [NE, TOK])
                    bds = sb(sr, "bds", [NE, DM])
                    tr = sb(sr, "tr", [128, NE])
                    S.dma('sp', [], ['ln1gb'], out=gb1[:].rearrange("p a d -> p (a d)"), in_=ln1.rearrange("a d -> (a d)").partition_broadcast(128))
                    S.dma('sp', [], ['wr'], out=wr[:], in_=w_r.rearrange("(c p) e -> p c e", p=128))
                    S.dma('sp', [], ['brb'], out=brb[:], in_=b_r[0, :].partition_broadcast(128))
                    S.dma('sp', [], ['bds'], out=bds[:], in_=b_dn[:, :])
                    hti = 0

                    def part1(i):
                        nonlocal hti
                        fb = i % 2
                        layer_norm_tile(acc[:, i, :], h1f[fb][:], gb1, stats, mv, rs, tmp, [('acc', i)], [('h1f', fb)], 'ln1')
                        S.op('act', 'activation', [('h1f', fb)], [('h1b', i)], out=h1b[:, i, :], in_=h1f[fb][:], func=AF.Copy)
                        S.op('act', 'activation', [('h1f', fb)], [('acc', i)], out=acc[:, i, :], in_=h1f[fb][:], func=AF.Copy, scale=ALPHA)
                        for c in range(16):
                            pb = c % 4
                            S.op('pe', 'matmul', [('h1f', fb), 'cf'], [('ps', pb)], PS[pb][:, 0:128], lhsT=h1f[fb][:, c * 128:(c + 1) * 128], rhs=identf, start=True, stop=True)
                            tb = hti % 4
                            hti += 1
                            S.op('act' if c % 2 else 'dve', 'activation' if c % 2 else 'tensor_copy', [('ps', pb)], [('hT', tb)],
                                 **(dict(out=hT[tb][:], in_=PS[pb][:, 0:128], func=AF.Copy) if c % 2 else dict(out=hT[tb][:], in_=PS[pb][:, 0:128])))
                            S.op('pe', 'matmul', [('hT', tb), 'wr'], [('ps', 4 + i % 2)], PS[4 + i % 2][:, 0:NE], lhsT=hT[tb][:], rhs=wr[:, c, :], start=(c == 0), stop=(c == 15))

                    def part2(i):
                        S.op('dve', 'tensor_tensor', [('ps', 4 + i % 2), 'brb'], ['lg'], out=lg[:], in0=PS[4 + i % 2][:, 0:NE], in1=brb[:], op=ALU.add)
                        S.op('dve', 'max', ['lg'], ['t8'], out=t8[:], in_=lg[:])
                        S.op('dve', 'tensor_scalar', ['lg', 't8'], [('selt', i)], out=selt[:, i, :], in0=lg[:], scalar1=t8[:, 3:4], scalar2=None, op0=ALU.is_ge)
                        S.op('dve', 'tensor_scalar', ['t8'], ['nmx'], out=nmx[:], in0=t8[:, 0:1], scalar1=-1.0, scalar2=None, op0=ALU.mult)
                        S.op('act', 'activation', ['lg', 'nmx'], ['exr'], out=exr[:], in_=lg[:], func=AF.Exp, bias=nmx[:, 0:1], scale=1.0)
                        S.op('dve', 'tensor_tensor', ['exr', ('selt', i)], ['exr'], out=exr[:], in0=exr[:], in1=selt[:, i, :], op=ALU.mult)
                        S.op('dve', 'tensor_reduce', ['exr'], ['den'], out=den[:], in_=exr[:], axis=AX.X, op=ALU.add)
                        S.op('dve', 'reciprocal', ['den'], ['den'], out=den[:], in_=den[:])
                        S.op('dve', 'tensor_scalar', ['exr', 'den'], [('gate', i)], out=gate[:, i, :], in0=exr[:], scalar1=den[:, 0:1], scalar2=None, op0=ALU.mult)
                        S.op('dve', 'tensor_copy', [('selt', i)], [('selb', i)], out=selb[:, i, :], in_=selt[:, i, :])

                    for i in range(NT + 1):
                        if i < NT:
                            part1(i)
                        if i >= 1:
                            part2(i - 1)
                    for i in range(NT):
                        for i2 in range(i + 1):
                            S.op('pe', 'matmul', [('selb', i2), 'cb'], [('ps', 5)], PS[5][:, 0:NE], lhsT=(utrib if i2 == i else onesb), rhs=selb[:, i2, :], start=(i2 == 0), stop=(i2 == i))
                        S.op('dve', 'tensor_tensor', [('ps', 5), ('selt', i)], ['tr'], out=tr[:], in0=PS[5][:, 0:NE], in1=selt[:, i, :], op=ALU.mult)
                        S.op('dve', 'scalar_tensor_tensor', ['tr', ('selt', i)], [('rankm', i)], out=rankm[:, i, :], in0=selt[:, i, :], scalar=-1.0, in1=tr[:], op0=ALU.add, op1=ALU.add)
                        for i2 in range(i + 1):
                            S.op('pe', 'matmul', [('selb', i2), 'cb'], [('ps', 6)], PS[6][0:NE, 0:128], lhsT=selb[:, i2, :], rhs=(utrib if i2 == i else onesb), start=(i2 == 0), stop=(i2 == i))
                        S.op('pe', 'matmul', [('selb', i), 'cb'], [('ps', 7)], PS[7][0:NE, 0:128], lhsT=selb[:, i, :], rhs=identb, start=True, stop=True)
                        S.op('pe', 'matmul', [('gate', i), 'cf'], [('ps', 3)], PS[3][0:NE, 0:128], lhsT=gate[:, i, :], rhs=identf, start=True, stop=True)
                        ts_ = slice(i * 128, (i + 1) * 128)
                        S.op('act', 'activation', [('ps', 7)], [('slT', i)], out=slT[:, ts_], in_=PS[7][0:NE, 0:128], func=AF.Copy)
                        S.op('act', 'activation', [('ps', 3)], [('gateT', i)], out=gateT[:, ts_], in_=PS[3][0:NE, 0:128], func=AF.Copy)
                        S.op('dve', 'tensor_tensor', [('ps', 6), ('slT', i)], [('rkT', i)], out=rkT[:, ts_], in0=PS[6][0:NE, 0:128], in1=slT[:, ts_], op=ALU.mult)
                        S.op('dve', 'scalar_tensor_tensor', [('rkT', i), ('slT', i)], [('rkT', i)], out=rkT[:, ts_], in0=slT[:, ts_], scalar=-1.0, in1=rkT[:, ts_], op0=ALU.add, op1=ALU.add)
                    S.dma('sp', [('rkT', i) for i in range(NT)], ['RK'], out=RK[:, :], in_=rkT[:])
                    S.dma('sp', [('gateT', i) for i in range(NT)], ['GT'], out=GT[:, :], in_=gateT[:])
                    k = 0
                    for i in range(NT):
                        for cc in range(4):
                            pb = k % 4
                            k += 1
                            S.op('pe', 'matmul', [('gateT', i), 'bds'], [('ps', pb)], PS[pb][:], lhsT=gateT[:, i * 128:(i + 1) * 128], rhs=bds[:, cc * 512:(cc + 1) * 512], start=True, stop=True)
                            S.op('dve', 'tensor_tensor', [('ps', pb), ('acc', i)], [('acc', i)], out=acc[:, i, cc * 512:(cc + 1) * 512], in0=PS[pb][:], in1=acc[:, i, cc * 512:(cc + 1) * 512], op=ALU.add)
                    S.barrier()
                    if stop == 6:
                        dump_f(accraw[:].bitcast(F32), 0, 16384)
                        S.dma('sp', [], ['dbgb6'], out=dbgb[:, 0:16384], in_=h1b[:].rearrange('p a d -> p (a d)'))
                        raise _Stop()

                with ExitStack() as sd:
                    RW = 7
                    wb = [sb(sd, "wb%d" % i, [128, 16, 256], BF16) for i in range(RW)]
                    wi = 0
                    xg = sb(sd, "xg", [128, 16, CAP], BF16)
                    actT = sb(sd, "actT", [128, 16, CAP], BF16)
                    selE = [sb(sd, "selE%d" % i, [128, NT, CAP], BF16) for i in range(1)]
                    selG = [sb(sd, "selG%d" % i, [128, 2, TOK], BF16) for i in range(1)]
                    rbc = [sb(sd, "rbc%d" % i, [128, TOK]) for i in range(1)]
                    gbc = [sb(sd, "gbc%d" % i, [128, TOK]) for i in range(1)]
                    yb = [sb(sd, "yb%d" % i, [128, 2, 256], BF16) for i in range(2)]
                    g1 = [sb(sd, "g1%d" % i, [128, CAP]) for i in range(2)]
                    sgm = [sb(sd, "sgm%d" % i, [128, CAP]) for i in range(2)]
                    u1 = [sb(sd, "u1%d" % i, [128, CAP]) for i in range(2)]
                    gui = 0
                    dni = 0
                    xgi = 0
                    pri = 0
                    pend = None
                    for e in range(NE):
                        eb = 0
                        S.dma('sp', ['RK'], [('rbc', eb)], out=rbc[eb][:], in_=RK[e, :].partition_broadcast(128))
                        S.dma('sp', ['GT'], [('gbc', eb)], out=gbc[eb][:], in_=GT[e, :].partition_broadcast(128))
                        for i in range(NT):
                            S.op('dve', 'tensor_scalar', ['cf', ('rankm', i)], [('selE', eb, i)], out=selE[eb][:, i, :], in0=iota_row[:, 0:CAP], scalar1=rankm[:, i, e:e + 1], scalar2=None, op0=ALU.is_equal)
                        for j in range(2):
                            mj = MJ[j]
                            S.op('dve', 'scalar_tensor_tensor', [('rbc', eb), ('gbc', eb), 'c2'], [('selG', eb, j)], out=selG[eb][0:mj, j, :], in0=rbc[eb][0:mj, :],
                                 scalar=iota_p2[0:mj, j:j + 1], in1=gbc[eb][0:mj, :], op0=ALU.is_equal, op1=ALU.mult)
                        for c in range(16):
                            hbk = xgi % 2
                            xgi += 1
                            gp = PS[hbk][:, 0:CAP]
                            for i in range(NT):
                                S.op('pe', 'matmul', [('h1b', i), ('selE', eb, i)], [('psg', hbk)], gp, lhsT=h1b[:, i, c * 128:(c + 1) * 128], rhs=selE[eb][:, i, :], start=(i == 0), stop=(i == NT - 1))
                            S.op('act', 'activation', [('psg', hbk)], [('xg', c)], out=xg[:, c, :], in_=gp, func=AF.Copy)
                        xgk = [('xg', c) for c in range(16)]
                        w_gu_v = w_gu[e].rearrange("(c p) f -> p c f", p=128)
                        for q in range(8):
                            s1 = wi % RW
                            s2 = (wi + 1) % RW
                            wi += 2
                            S.dma('pool', [], [('wb', s1)], out=wb[s1][:], in_=w_gu_v[:, :, q * 256:(q + 1) * 256])
                            S.dma('pool', [], [('wb', s2)], out=wb[s2][:], in_=w_gu_v[:, :, DM + q * 256:DM + (q + 1) * 256])
                            for j in range(2):
                                fc = q * 2 + j
                                pb = 2 + pri % 2
                                pri += 1
                                tb = pri % 2
                                gps = PS[pb][:, 0:CAP]
                                ups = PS[pb][:, 256:256 + CAP]
                                for c in range(16):
                                    S.op('pe', 'matmul', [('wb', s1)] + xgk, [('psgu', pb, 0)], gps, lhsT=wb[s1][:, c, j * 128:(j + 1) * 128], rhs=xg[:, c, :], start=(c == 0), stop=(c == 15))
                                for c in range(16):
                                    S.op('pe', 'matmul', [('wb', s2)] + xgk, [('psgu', pb, 1)], ups, lhsT=wb[s2][:, c, j * 128:(j + 1) * 128], rhs=xg[:, c, :], start=(c == 0), stop=(c == 15))
                                bg = bgs[:, e * 32 + fc:e * 32 + fc + 1]
                                bu = bgs[:, e * 32 + 16 + fc:e * 32 + 16 + fc + 1]
                                S.op('dve', 'tensor_scalar', [('psgu', pb, 0), ('psgu', pb, 1), 'bgs'], [('g1', tb)], out=g1[tb][:], in0=gps, scalar1=bg, scalar2=7.0, op0=ALU.add, op1=ALU.min)
                                S.op('act', 'activation', [('g1', tb)], [('sgm', tb)], out=sgm[tb][:], in_=g1[tb][:], func=AF.Sigmoid, scale=1.702)
                                S.op('dve', 'tensor_scalar', [('psgu', pb, 1), 'bgs'], [('u1', tb)], out=u1[tb][:], in0=ups, scalar1=bu, scalar2=7.0, op0=ALU.add, op1=ALU.min)
                                S.op('dve', 'tensor_scalar', [('u1', tb)], [('u1', tb)], out=u1[tb][:], in0=u1[tb][:], scalar1=-7.0, scalar2=1.0, op0=ALU.max, op1=ALU.add)
                                if pend is not None:
                                    ptb, pfc = pend
                                    S.op('dve', 'tensor_tensor', [('g1', ptb), ('sgm', ptb)], [('g1', ptb)], out=g1[ptb][:], in0=g1[ptb][:], in1=sgm[ptb][:], op=ALU.mult)
                                    S.op('dve', 'tensor_tensor', [('g1', ptb), ('u1', ptb)], [('actT', pfc)], out=actT[:, pfc, :], in0=g1[ptb][:], in1=u1[ptb][:], op=ALU.mult)
                                pend = (tb, fc)
                        ptb, pfc = pend
                        S.op('dve', 'tensor_tensor', [('g1', ptb), ('sgm', ptb)], [('g1', ptb)], out=g1[ptb][:], in0=g1[ptb][:], in1=sgm[ptb][:], op=ALU.mult)
                        S.op('dve', 'tensor_tensor', [('g1', ptb), ('u1', ptb)], [('actT', pfc)], out=actT[:, pfc, :], in0=g1[ptb][:], in1=u1[ptb][:], op=ALU.mult)
                        pend = None
                        ak = [('actT', fc) for fc in range(16)]
                        w_dn_v = w_dn[e].rearrange("(c p) f -> p c f", p=128)
                        for r in range(8):
                            db = dni % 2
                            dni += 1
                            s3 = wi % RW
                            wi += 1
                            S.dma('pool', [], [('wb', s3)], out=wb[s3][:], in_=w_dn_v[:, :, r * 256:(r + 1) * 256])
                            for j in range(2):
                                mj = MJ[j]
                                yp = PS[4 + j][0:mj, 0:256]
                                for c in range(16):
                                    S.op('pe', 'matmul', [('wb', s3)] + ak, [('psy', j)], yp, lhsT=actT[:, c, j * 128:j * 128 + mj], rhs=wb[s3][:, c, :], start=(c == 0), stop=(c == 15))
                                S.op('act', 'activation', [('psy', j)], [('yb', db, j)], out=yb[db][0:mj, j, :], in_=yp, func=AF.Copy)
                            for i in range(NT):
                                sp_ = PS[6 + i % 2][:, 0:256]
                                for j in range(2):
                                    S.op('pe', 'matmul', [('selG', eb, j), ('yb', db, j)], [('pss', i % 2)], sp_, lhsT=selG[eb][0:MJ[j], j, i * 128:(i + 1) * 128], rhs=yb[db][0:MJ[j], j, :], start=(j == 0), stop=(j == 1))
                                S.op('dve', 'tensor_tensor', [('pss', i % 2), ('acc', i, r)], [('acc', i, r)], out=acc[:, i, r * 256:(r + 1) * 256], in0=sp_, in1=acc[:, i, r * 256:(r + 1) * 256], op=ALU.add)
                    S.barrier()
                    if stop == 7:
                        dump_f(accraw[:].bitcast(F32), 0, 16384)
                        raise _Stop()

                with ExitStack() as se:
                    gb2 = sb(se, "gb2", [128, 2, DM])
                    stats = sb(se, "stats2", [128, 4, 6])
                    mv = sb(se, "mv2", [128, 2])
                    rs = sb(se, "rs2", [128, 1])
                    tmp = sb(se, "tmp2", [128, DM])
                    ob_ = [sb(se, "ob%d" % i, [128, DM]) for i in range(2)]
                    S.dma('sp', [], ['ln2gb'], out=gb2[:].rearrange("p a d -> p (a d)"), in_=ln2.rearrange("a d -> (a d)").partition_broadcast(128))
                    for i in range(NT):
                        fb = i % 2
                        layer_norm_tile(acc[:, i, :], ob_[fb][:], gb2, stats, mv, rs, tmp, [('acc', i)], [('ob', fb)], 'ln2')
                        S.dma('sp', [('ob', fb)], [('out', i)], out=out[i * 128:(i + 1) * 128, :], in_=ob_[fb][:])
        except _Stop:
            pass
        S.finalize(st)
    return nc


_CACHE = {}


def _consts():
    p = np.arange(128)
    cst = np.zeros((128, 1024), np.float32)
    cst[:, 0:128] = np.eye(128)
    cst[:, 128:256] = (p[:, None] <= p[None, :])
    cst[:, 256:384] = (p[:, None] < p[None, :])
    cst[:, 384:512] = 1.0
    cst[:, 512:768] = np.arange(256)[None, :]
    c2 = np.zeros((128, 64), np.float32)
    c2[:, 0] = p
    c2[:, 1] = p + 128
    for h in range(NH):
        for kk in range(2):
            c2[:, 2 + 2 * h + kk] = SLOPES[h] * (p + 128 * kk - 128)
    for i in range(NT):
        c2[:, 18 + i] = 128 - (i % 2) * 128 - p
    return cst, c2


def kernel(x, w_in, conv_w, conv_b, conv_ln_g, conv_ln_b, w_out, ln1_g, ln1_b,
           w_router, b_router, w_gate_up, b_gate_up, w_down, b_down, ln2_g, ln2_b):
    f = lambda a: np.ascontiguousarray(np.asarray(a, dtype=np.float32))
    x2 = f(x)[0]
    xT = np.ascontiguousarray(x2.T)
    if 'nc' not in _CACHE:
        _CACHE['nc'] = build()
    nc = _CACHE['nc']
    cst, c2 = _consts()
    convw = np.ascontiguousarray(f(conv_w).T.reshape(8, 128, 31).transpose(1, 0, 2).reshape(128, 8 * 31))
    cv = np.concatenate([f(v).reshape(8, 128).T for v in (conv_b, conv_ln_g, conv_ln_b)], axis=1)
    bgu = np.ascontiguousarray(f(b_gate_up).reshape(NE, 32, 128).transpose(2, 0, 1).reshape(128, NE * 32))
    shared = dict(
        xT=xT, w_in=f(w_in), convw=convw, convv=np.ascontiguousarray(cv), w_out=f(w_out),
        ln1=np.stack([f(ln1_g), f(ln1_b)]), ln2=np.stack([f(ln2_g), f(ln2_b)]),
        w_r=f(w_router), b_r=f(b_router).reshape(1, NE), w_gu=f(w_gate_up), bgu=bgu,
        w_dn=f(w_down), b_dn=f(b_down), cst=cst, cst2=c2)
    in_maps = []
    p = np.arange(128)
    for c in range(NCORES):
        t0 = c * TOK
        xto = np.zeros((DM, 1056), np.float32)
        lo = max(t0 - 32, 0)
        xto[:, 1056 - (t0 + TOK - lo):] = xT[:, lo:t0 + TOK]
        tq = t0 + np.arange(NT)[None, :, None] * 128 + p[:, None, None]
        n = np.arange(32)[None, None, :]
        past = n < (tq // 256)
        dt = np.where(past, -(tq - 256 * n - 128), 0).astype(np.float32).reshape(128, 256)
        fp = np.where(past, 0.0, NEG).astype(np.float32).reshape(128, 256)
        m = dict(shared)
        m.update(xTo=xto, xo=np.ascontiguousarray(x2[t0:t0 + TOK]), dtab=np.ascontiguousarray(dt), fptab=np.ascontiguousarray(fp))
        in_maps.append(m)
    res = run_bass_kernel_spmd(nc, in_maps, core_ids=list(range(NCORES)))
    outp = np.concatenate([r["out"] for r in res.results], axis=0)
    return outp.reshape(1, SEQ, DM).astype(np.float32)
```
